# Optimizing a Trainium2 kernel written in Bass

```python
import math
import jax
import jax.numpy as jnp
from jax import lax
import numpy as np

D_MODEL = 1024
BATCH = 4
SEQ = 4096
DEPTH = 1

D_MIX = D_MODEL
D_S5 = D_MIX // 2
S5_GROUP = 16
N_S5_GROUPS = D_S5 // S5_GROUP
S5_STATE = 64
D_GDN = D_MIX - D_S5
GDN_HEAD_DIM = 128
N_GDN_HEADS = D_GDN // GDN_HEAD_DIM
CONV_WIDTH = 4
CHUNK = 64
D_IN = D_S5 + 4 * D_GDN + 2 * N_GDN_HEADS
N_EXPERT_GROUPS = 4
EXPERTS_PER_GROUP = 8
N_EXPERTS = N_EXPERT_GROUPS * EXPERTS_PER_GROUP
TOP_K_IN_GROUP = 2
D_EXPERT = D_MODEL // 4
N_MOD = 6
EPS = 1e-6

kernel_name = 'hymba_s5_gdn_hmoe_adaln_block'


def rmsnorm(x, g):
    x32 = x.astype(jnp.float32)
    y = x32 * lax.rsqrt(jnp.mean(x32 * x32, axis=-1, keepdims=True) + EPS)
    return (y * g.astype(jnp.float32)).astype(x.dtype)


def l2norm(x):
    return x * lax.rsqrt(jnp.sum(x * x, axis=-1, keepdims=True) + EPS)


def modulate(h, shift, scale):
    return h * (1.0 + scale[:, None, :]) + shift[:, None, :]


def _linear_recurrence_combine(e1, e2):
    a1, b1 = e1
    a2, b2 = e2
    return a1 * a2, a2 * b1 + b2


def s5_mixer(u, lam_re, lam_im, log_step, b_re, b_im, c_re, c_im, d_skip, w_glu, b_glu):
    f32 = jnp.float32
    bsz, seq, _ = u.shape
    u32 = u.astype(f32)
    ug = u32.reshape(bsz, seq, N_S5_GROUPS, S5_GROUP)
    lam = lax.complex(lam_re.astype(f32), lam_im.astype(f32))
    step = jnp.exp(log_step.astype(f32))[:, None]
    lam_bar = jnp.exp(lam * step)
    b_c = lax.complex(b_re.astype(f32), b_im.astype(f32))
    b_bar = ((lam_bar - 1.0) / lam)[..., None] * b_c
    bu = lax.complex(jnp.einsum('gph,blgh->blgp', b_bar.real, ug),
                     jnp.einsum('gph,blgh->blgp', b_bar.imag, ug))
    a = jnp.broadcast_to(lam_bar, bu.shape)
    _, states = lax.associative_scan(_linear_recurrence_combine, (a, bu), axis=1)
    c_c = lax.complex(c_re.astype(f32), c_im.astype(f32))
    y = jnp.einsum('ghp,blgp->blgh', c_c, states).real.reshape(bsz, seq, D_S5)
    y = y + d_skip.astype(f32) * u32
    y = jax.nn.gelu(y)
    y = y * jax.nn.sigmoid(y @ w_glu.astype(f32) + b_glu.astype(f32))
    return y.astype(u.dtype)


def causal_depthwise_conv(x, w):
    k, ch = w.shape
    return lax.conv_general_dilated(x, w[:, None, :], window_strides=(1,),
                                    padding=[(k - 1, 0)],
                                    dimension_numbers=('NWC', 'WIO', 'NWC'),
                                    feature_group_count=ch)


def gated_delta_rule(q, k, v, g, beta):
    bsz, nh, seq, dk = q.shape
    dv = v.shape[-1]
    n_chunks = seq // CHUNK
    q = q.reshape(bsz, nh, n_chunks, CHUNK, dk)
    k = k.reshape(bsz, nh, n_chunks, CHUNK, dk)
    v = v.reshape(bsz, nh, n_chunks, CHUNK, dv)
    beta = beta.reshape(bsz, nh, n_chunks, CHUNK)
    g = jnp.cumsum(g.reshape(bsz, nh, n_chunks, CHUNK), axis=-1)
    idx = jnp.arange(CHUNK)
    causal = idx[:, None] >= idx[None, :]
    strict = idx[:, None] > idx[None, :]
    decay = jnp.exp(jnp.where(causal, g[..., :, None] - g[..., None, :], -jnp.inf))
    k_beta = k * beta[..., None]
    v_beta = v * beta[..., None]
    kkt = jnp.einsum('bhncd,bhnsd->bhncs', k_beta, k) * decay
    a_mat = jnp.where(strict, kkt, 0.0) + jnp.eye(CHUNK, dtype=q.dtype)
    rhs = jnp.concatenate([v_beta, k_beta * jnp.exp(g)[..., None]], axis=-1)
    sol = lax.linalg.triangular_solve(a_mat, rhs, left_side=True, lower=True,
                                      unit_diagonal=True)
    value, k_cumdecay = sol[..., :dv], sol[..., dv:]
    qk = jnp.einsum('bhncd,bhnsd->bhncs', q, k) * decay
    q_decay = q * jnp.exp(g)[..., None]
    k_tail = k * jnp.exp(g[..., -1:] - g)[..., None]
    g_last = jnp.exp(g[..., -1])

    def chunk_step(state, inp):
        qk_i, qd_i, kc_i, val_i, kt_i, gl_i = inp
        v_new = val_i - jnp.einsum('bhcd,bhde->bhce', kc_i, state)
        o_i = (jnp.einsum('bhcd,bhde->bhce', qd_i, state)
               + jnp.einsum('bhcs,bhse->bhce', qk_i, v_new))
        state = state * gl_i[..., None, None] + jnp.einsum('bhcd,bhce->bhde', kt_i, v_new)
        return state, o_i

    xs = tuple(jnp.moveaxis(t, 2, 0) for t in (qk, q_decay, k_cumdecay, value, k_tail, g_last))
    state0 = jnp.zeros((bsz, nh, dk, dv), q.dtype)
    _, o = lax.scan(chunk_step, state0, xs)
    return jnp.moveaxis(o, 0, 2).reshape(bsz, nh, seq, dv)


def gdn_mixer(qkv, z, a_gate, b_gate, conv_w, a_log, dt_bias, gdn_norm_g):
    f32 = jnp.float32
    bsz, seq, _ = qkv.shape
    qkv = jax.nn.silu(causal_depthwise_conv(qkv, conv_w)).astype(f32)
    q, k, v = jnp.split(qkv, 3, axis=-1)

    def heads(t):
        return t.reshape(bsz, seq, N_GDN_HEADS, GDN_HEAD_DIM).transpose(0, 2, 1, 3)

    q = l2norm(heads(q)) * (GDN_HEAD_DIM ** -0.5)
    k = l2norm(heads(k))
    v = heads(v)
    beta = jax.nn.sigmoid(b_gate.astype(f32)).transpose(0, 2, 1)
    g = (-jnp.exp(a_log.astype(f32))
         * jax.nn.softplus(a_gate.astype(f32) + dt_bias.astype(f32))).transpose(0, 2, 1)
    o = gated_delta_rule(q, k, v, g, beta).transpose(0, 2, 1, 3)
    zh = z.astype(f32).reshape(bsz, seq, N_GDN_HEADS, GDN_HEAD_DIM)
    o = rmsnorm(o, gdn_norm_g) * jax.nn.silu(zh)
    return o.reshape(bsz, seq, D_GDN).astype(z.dtype)


def hybrid_mixer(h, w_in, lam_re, lam_im, log_step, s5_b_re, s5_b_im, s5_c_re, s5_c_im,
                 s5_d, w_glu, b_glu, conv_w, a_log, dt_bias, gdn_norm_g, w_out):
    proj = h @ w_in
    splits = [D_S5, D_S5 + 3 * D_GDN, D_S5 + 4 * D_GDN, D_S5 + 4 * D_GDN + N_GDN_HEADS]
    u_s5, qkv, z, a_gate, b_gate = jnp.split(proj, splits, axis=-1)
    y_s5 = s5_mixer(u_s5, lam_re, lam_im, log_step, s5_b_re, s5_b_im, s5_c_re, s5_c_im,
                    s5_d, w_glu, b_glu)
    y_gdn = gdn_mixer(qkv, z, a_gate, b_gate, conv_w, a_log, dt_bias, gdn_norm_g)
    return jnp.concatenate([y_s5, y_gdn], axis=-1) @ w_out


def hier_moe(h, w_router_grp, w_router_exp, w_gate, w_up, w_down):
    f32 = jnp.float32
    bsz, seq, d = h.shape
    hf = h.reshape(bsz * seq, d)
    grp_logits = (hf @ w_router_grp).astype(f32)
    grp_prob = jax.nn.softmax(grp_logits, axis=-1)
    grp_idx = jnp.argmax(grp_logits, axis=-1)
    grp_onehot = jax.nn.one_hot(grp_idx, N_EXPERT_GROUPS, dtype=f32)
    p_grp = jnp.sum(grp_prob * grp_onehot, axis=-1)
    exp_logits = (hf @ w_router_exp).astype(f32).reshape(-1, N_EXPERT_GROUPS, EXPERTS_PER_GROUP)
    sel_logits = jnp.einsum('tg,tge->te', grp_onehot, exp_logits)
    top_val, top_idx = lax.top_k(sel_logits, TOP_K_IN_GROUP)
    w_k = jax.nn.softmax(top_val, axis=-1) * p_grp[:, None]
    expert_id = grp_idx[:, None] * EXPERTS_PER_GROUP + top_idx
    combine = jnp.einsum('tk,tke->te', w_k,
                         jax.nn.one_hot(expert_id, N_EXPERTS, dtype=f32)).astype(h.dtype)

    def expert_step(acc, ws):
        wg, wu, wd, gate_col = ws
        y = (jax.nn.silu(hf @ wg) * (hf @ wu)) @ wd
        return acc + gate_col[:, None] * y, None

    acc0 = jnp.zeros_like(hf)
    out, _ = lax.scan(expert_step, acc0, (w_gate, w_up, w_down, combine.T))
    return out.reshape(bsz, seq, d)


def setup_inputs(seed: int = 0) -> dict:
    key = jax.random.key(seed)
    ks = jax.random.split(key, 32)
    f32 = jnp.float32

    def nrm(k, shape, scale):
        return jax.random.normal(k, shape, f32) * scale

    nl = DEPTH
    gp = (nl, N_S5_GROUPS, S5_STATE)
    n_idx = jnp.arange(S5_STATE, dtype=f32)
    dt_init = jnp.exp(jax.random.uniform(ks[21], (nl, N_GDN_HEADS), f32,
                                         math.log(1e-3), math.log(1e-1)))
    return {
        'x': nrm(ks[0], (BATCH, SEQ, D_MODEL), 1.0),
        'c': nrm(ks[1], (BATCH, D_MODEL), 1.0),
        'norm1_g': 1.0 + nrm(ks[2], (nl, D_MODEL), 0.02),
        'norm2_g': 1.0 + nrm(ks[3], (nl, D_MODEL), 0.02),
        'w_ada': nrm(ks[4], (nl, D_MODEL, N_MOD * D_MODEL), 0.5 * D_MODEL ** -0.5),
        'b_ada': nrm(ks[5], (nl, N_MOD * D_MODEL), 0.02),
        'w_in': nrm(ks[6], (nl, D_MODEL, D_IN), D_MODEL ** -0.5),
        'lam_re': -0.5 + nrm(ks[7], gp, 0.01),
        'lam_im': math.pi * n_idx + nrm(ks[8], gp, 0.01),
        'log_step': jax.random.uniform(ks[9], (nl, N_S5_GROUPS), f32,
                                       math.log(1e-3), math.log(1e-1)),
        's5_b_re': nrm(ks[10], (nl, N_S5_GROUPS, S5_STATE, S5_GROUP), (2 * S5_GROUP) ** -0.5),
        's5_b_im': nrm(ks[11], (nl, N_S5_GROUPS, S5_STATE, S5_GROUP), (2 * S5_GROUP) ** -0.5),
        's5_c_re': nrm(ks[12], (nl, N_S5_GROUPS, S5_GROUP, S5_STATE), S5_STATE ** -0.5),
        's5_c_im': nrm(ks[13], (nl, N_S5_GROUPS, S5_GROUP, S5_STATE), S5_STATE ** -0.5),
        's5_d': nrm(ks[14], (nl, D_S5), 0.5),
        'w_glu': nrm(ks[15], (nl, D_S5, D_S5), D_S5 ** -0.5),
        'b_glu': nrm(ks[16], (nl, D_S5), 0.02),
        'conv_w': nrm(ks[17], (nl, CONV_WIDTH, 3 * D_GDN), CONV_WIDTH ** -0.5),
        'a_log': jnp.log(jax.random.uniform(ks[18], (nl, N_GDN_HEADS), f32, 1.0, 16.0)),
        'dt_bias': dt_init + jnp.log(-jnp.expm1(-dt_init)),
        'gdn_norm_g': 1.0 + nrm(ks[19], (nl, GDN_HEAD_DIM), 0.02),
        'w_out': nrm(ks[20], (nl, D_MIX, D_MODEL), D_MIX ** -0.5),
        'w_router_grp': nrm(ks[22], (nl, D_MODEL, N_EXPERT_GROUPS), D_MODEL ** -0.5),
        'w_router_exp': nrm(ks[23], (nl, D_MODEL, N_EXPERTS), D_MODEL ** -0.5),
        'w_gate': nrm(ks[24], (nl, N_EXPERTS, D_MODEL, D_EXPERT), D_MODEL ** -0.5),
        'w_up': nrm(ks[25], (nl, N_EXPERTS, D_MODEL, D_EXPERT), D_MODEL ** -0.5),
        'w_down': nrm(ks[26], (nl, N_EXPERTS, D_EXPERT, D_MODEL), D_EXPERT ** -0.5),
        'normf_g': 1.0 + nrm(ks[27], (D_MODEL,), 0.02),
    }


def reference(x, c, norm1_g, norm2_g, w_ada, b_ada, w_in, lam_re, lam_im, log_step,
              s5_b_re, s5_b_im, s5_c_re, s5_c_im, s5_d, w_glu, b_glu, conv_w, a_log,
              dt_bias, gdn_norm_g, w_out, w_router_grp, w_router_exp, w_gate, w_up,
              w_down, normf_g):
    h = x
    c_act = jax.nn.silu(c)
    for l in range(DEPTH):
        mod = c_act @ w_ada[l] + b_ada[l]
        sh1, sc1, gt1, sh2, sc2, gt2 = jnp.split(mod, N_MOD, axis=-1)
        a_in = modulate(rmsnorm(h, norm1_g[l]), sh1, sc1)
        mix = hybrid_mixer(a_in, w_in[l], lam_re[l], lam_im[l], log_step[l], s5_b_re[l],
                           s5_b_im[l], s5_c_re[l], s5_c_im[l], s5_d[l], w_glu[l], b_glu[l],
                           conv_w[l], a_log[l], dt_bias[l], gdn_norm_g[l], w_out[l])
        h = h + gt1[:, None, :] * mix
        m_in = modulate(rmsnorm(h, norm2_g[l]), sh2, sc2)
        moe = hier_moe(m_in, w_router_grp[l], w_router_exp[l], w_gate[l], w_up[l], w_down[l])
        h = h + gt2[:, None, :] * moe
    return rmsnorm(h, normf_g)
```

```python
import math
import os
from contextlib import ExitStack

import numpy as np
import concourse.bass as bass
import concourse.mybir as mybir
from concourse.bass_utils import run_bass_kernel_spmd

F32 = mybir.dt.float32
BF16 = mybir.dt.bfloat16
I32 = mybir.dt.int32
AF = mybir.ActivationFunctionType
ALU = mybir.AluOpType
AX = mybir.AxisListType

ENGS = ["sp", "act", "dve", "pool", "pe"]
SEM_SPAN = 30000


class _Op:
    __slots__ = ("eng", "fn", "deps", "dsem", "dcount", "need_sig", "sig", "dma_waits", "alld", "cost", "lat",
                 "idx", "succ", "npred", "prio", "rt", "fin", "grp")

    def __init__(self, eng, fn, dsem):
        self.eng = eng
        self.fn = fn
        self.deps = []
        self.dma_waits = []
        self.alld = []
        self.dsem = dsem
        self.dcount = 0
        self.need_sig = False
        self.sig = None
        self.cost = 0.1
        self.lat = 0.0
        self.grp = None


HOP = 0.7
SEG_HOP = {"G": 0.85, "S_setup": 1.1, "S_p1": 1.1, "S_p23": 1.1}
ACT_SWITCH = 1.3


class Prog:
    def __init__(self, nc):
        self.nc = nc
        self.cur = {e: [] for e in ENGS}
        self.segs = []
        self.last_w = {}
        self.readers = {}
        self.dcount = {}
        self.last_dma = {}
        self.nops = 0
        self.sched = True

    def _dep(self, op, tok):
        if tok is not None and tok is not op:
            op.alld.append(tok)

    def op(self, eng, fn, r=(), w=(), dsem=None, cost=0.1, lat=0.0, grp=None):
        o = _Op(eng, fn, dsem)
        o.cost = cost
        o.lat = lat
        o.grp = grp
        o.idx = self.nops
        self.nops += 1
        for k in r:
            self._dep(o, self.last_w.get(k))
        for k in w:
            self._dep(o, self.last_w.get(k))
            for t in self.readers.get(k, ()):
                self._dep(o, t)
        if dsem is not None:
            self.dcount[dsem] = self.dcount.get(dsem, 0) + 16
            o.dcount = self.dcount[dsem]
            prev = self.last_dma.get(dsem)
            if prev is not None:
                assert prev.eng == eng, f"dsem {dsem} used from two queues"
                self._dep(o, prev)
            self.last_dma[dsem] = o
        for k in r:
            self.readers.setdefault(k, []).append(o)
        for k in w:
            self.last_w[k] = o
            self.readers[k] = []
        self.cur[eng].append(o)
        return o

    def barrier(self, name=None):
        self.segs.append((self.cur, dict(self.dcount), name))
        self.cur = {e: [] for e in ENGS}
        self.last_w.clear()
        self.readers.clear()
        self.last_dma.clear()

    def _schedule(self, seg, HOP=HOP):
        import heapq
        ops = [o for e in ENGS for o in seg[e]]
        if not ops:
            return seg
        ops.sort(key=lambda o: o.idx)
        inseg = set(id(o) for o in ops)
        for o in ops:
            o.succ = []
        for o in ops:
            seen = set()
            preds = []
            for d in o.alld:
                if id(d) in inseg and id(d) not in seen:
                    seen.add(id(d))
                    preds.append(d)
            o.alld = preds
            o.npred = len(preds)
            for d in preds:
                d.succ.append(o)
        for o in reversed(ops):
            m = 0.0
            for s in o.succ:
                h = 0.0 if (o.eng == "pe" and s.eng == "pe") else HOP
                if s.prio + h > m:
                    m = s.prio + h
            o.prio = o.cost + o.lat + m
            o.rt = 0.0
        free = {e: 0.0 for e in ENGS}
        pend = {e: [] for e in ENGS}
        ready = {e: [] for e in ENGS}
        order = {e: [] for e in ENGS}
        act_cur = [None]
        for o in ops:
            if o.npred == 0:
                heapq.heappush(pend[o.eng], (0.0, -o.prio, o.idx, o))
        n = len(ops)
        while n:
            best = None
            for e in ENGS:
                pe_, re_ = pend[e], ready[e]
                f = free[e]
                while pe_ and pe_[0][0] <= f:
                    it = heapq.heappop(pe_)
                    heapq.heappush(re_, (it[1], it[2], it[3]))
                if re_:
                    c = f
                elif pe_:
                    c = pe_[0][0]
                else:
                    continue
                if best is None or c < best[0]:
                    best = (c, e)
            c, e = best
            if ready[e]:
                if e == "act" and len(ready[e]) > 1:
                    top = heapq.nsmallest(6, ready[e])
                    pick = top[0]
                    if pick[2].grp is not None and pick[2].grp != act_cur[0]:
                        for it in top[1:]:
                            if (it[2].grp is None or it[2].grp == act_cur[0]) and it[0] <= pick[0] + 4.0:
                                pick = it
                                break
                    ready[e].remove(pick)
                    heapq.heapify(ready[e])
                    o = pick[2]
                else:
                    o = heapq.heappop(ready[e])[2]
            else:
                o = heapq.heappop(pend[e])[3]
            st_ = max(free[e], o.rt)
            cost = o.cost
            if e == "act" and o.grp is not None and o.grp != act_cur[0]:
                cost += ACT_SWITCH
                act_cur[0] = o.grp
            o.fin = st_ + cost
            free[e] = o.fin
            order[e].append(o)
            n -= 1
            for s in o.succ:
                h = 0.0 if (e == "pe" and s.eng == "pe") else HOP
                t_ = o.fin + o.lat + h
                if t_ > s.rt:
                    s.rt = t_
                s.npred -= 1
                if s.npred == 0:
                    heapq.heappush(pend[s.eng], (s.rt, -s.prio, s.idx, s))
        self.est_us = getattr(self, "est_us", 0.0) + max(free.values())
        return order

    def emit(self, stack):
        nc = self.nc
        if any(self.cur[e] for e in ENGS):
            self.barrier()
        final = {e: [] for e in ENGS}
        for seg, dc, name in self.segs:
            order = self._schedule(seg, SEG_HOP.get(name, HOP)) if self.sched else seg
            for e in ENGS:
                for o in order[e]:
                    for d in o.alld:
                        if d.dsem is not None:
                            if d.eng == o.eng and o.dsem == d.dsem:
                                continue
                            o.dma_waits.append((d.dsem, d.dcount))
                        elif d.eng == "pe" and o.eng == "pe":
                            continue
                        else:
                            d.need_sig = True
                            o.deps.append(d)
                    final[e].append(o)
            lasts = {}
            for e in ENGS:
                for o in reversed(final[e]):
                    if o.fn is not None and o.dsem is None:
                        lasts[e] = o
                        break
            for e in ENGS:
                f = _Op(e, None, None)
                for e2, o in lasts.items():
                    if e2 != e:
                        o.need_sig = True
                        f.deps.append(o)
                for name, cnt in dc.items():
                    f.dma_waits.append((name, cnt))
                final[e].append(f)
        esems = {}
        for e in ENGS:
            n = 0
            for o in final[e]:
                if o.need_sig and o.fn is not None and o.dsem is None:
                    o.sig = n
                    n += 1
            nsem = max(1, (n + SEM_SPAN - 1) // SEM_SPAN)
            esems[e] = [stack.enter_context(nc.semaphore(f"s_{e}{i}")) for i in range(nsem)]
        dsems = {name: stack.enter_context(nc.semaphore(f"d_{name}")) for name in self.dcount}
        block = stack.enter_context(nc.Block())

        def run(ename, eng):
            waited = {}
            for o in final[ename]:
                for d in o.deps:
                    if d.sig is None:
                        continue
                    key = (d.eng, d.sig // SEM_SPAN)
                    val = d.sig % SEM_SPAN + 1
                    if waited.get(key, 0) >= val:
                        continue
                    waited[key] = val
                    eng.wait_ge(esems[d.eng][key[1]], val)
                for (name, val) in o.dma_waits:
                    key = ("dma", name)
                    if waited.get(key, 0) >= val:
                        continue
                    waited[key] = val
                    eng.wait_ge(dsems[name], val)
                if o.fn is None:
                    continue
                ins = o.fn(eng)
                if o.dsem is not None:
                    ins.then_inc(dsems[o.dsem], 16)
                elif o.sig is not None:
                    ins.then_inc(esems[ename][o.sig // SEM_SPAN], 1)

        @block.sync
        def _(e):
            run("sp", e)

        @block.scalar
        def _(e):
            run("act", e)

        @block.vector
        def _(e):
            run("dve", e)

        @block.gpsimd
        def _(e):
            run("pool", e)

        @block.tensor
        def _(e):
            run("pe", e)


class Arena:
    def __init__(self, ap, n):
        self.ap = ap
        self.n = n
        self.off = 0

    def f32(self, n):
        a = self.ap[:, self.off:self.off + n]
        self.off += n
        assert self.off <= self.n, f"arena overflow {self.off} > {self.n}"
        return a

    def bf16(self, n):
        m = (n + 1) // 2
        return self.f32(m).bitcast(BF16)

    def i32(self, n):
        return self.f32(n).bitcast(I32)


D = 1024
NTOK = 4096
NOWN = 2048
EPS = 1e-6
TWO_PI = 2.0 * math.pi
PI_LO = 3.14159
ARENA_N = 53200


def dram_ap(t, offset, dims):
    return bass.AP(t.tensor, t.offset + offset, [list(d) for d in dims])


def build(phases=("0", "S", "G", "C"), dbg=()):
    nc = bass.Bass("TRN2", target_bir_lowering=False)

    def din(name, shape):
        return nc.dram_tensor(name, list(shape), F32, kind="ExternalInput").ap()

    xs = din("xs", [NTOK, D])
    flag_d = din("flag", [128, 1])
    cb = din("cb", [1, D])
    norm1_g = din("norm1_g", [1, D])
    norm2_g = din("norm2_g", [1, D])
    normf_g = din("normf_g", [1, D])
    w_ada = din("w_ada", [D, 6 * D])
    b_ada = din("b_ada", [6 * D])
    w_in = din("w_in", [D, 2568])
    lam_re = din("lam_re", [32, 64])
    lam_im = din("lam_im", [32, 64])
    log_step = din("log_step", [32])
    s5_b_re = din("s5_b_re", [32, 64, 16])
    s5_b_im = din("s5_b_im", [32, 64, 16])
    s5_c_re = din("s5_c_re", [32, 16, 64])
    s5_c_im = din("s5_c_im", [32, 16, 64])
    s5_d = din("s5_d", [512])
    w_glu = din("w_glu", [512, 512])
    b_glu = din("b_glu", [512])
    conv_w = din("conv_w", [4, 1536])
    a_log = din("a_log", [1, 4])
    dt_bias = din("dt_bias", [1, 4])
    gdn_norm_g = din("gdn_norm_g", [1, 128])
    w_out = din("w_out", [D, D])
    w_rg = din("w_router_grp", [D, 4])
    w_re = din("w_router_exp", [D, 32])
    w_gate = din("w_gate", [32, D, 256])
    w_up = din("w_up", [32, D, 256])
    w_down = din("w_down", [32, 256, D])
    out_d = nc.dram_tensor("out", [NOWN, D], F32, kind="ExternalOutput").ap()
    dbg_out = {}

    st = ExitStack()
    with st:
        arena_t = st.enter_context(nc.sbuf_tensor("arena", [128, ARENA_N], F32))
        A = Arena(arena_t[:], ARENA_N)
        PS = [st.enter_context(nc.psum_tensor(f"psb{i}", [128, 512], F32))[:] for i in range(8)]
        PSK = [f"ps{i}" for i in range(8)]
        P = Prog(nc)
        uid = [0]

        def fsz(ap):
            n = 1
            for s in ap.shape[1:]:
                n *= s
            return n

        ACT_GRP = {AF.Exp: "E", AF.Ln: "E", AF.Sigmoid: "G", AF.Silu: "S", AF.Sqrt: "Q", AF.Sin: "N", AF.Gelu_apprx_tanh: "U"}

        def c_dve(out):
            return 0.07 + 0.00105 * fsz(out)

        def c_pool(out):
            return 0.12 + 0.0022 * fsz(out)

        def c_act(out):
            return 0.2 + 0.00075 * fsz(out)

        def c_eng(eng, out):
            return {"dve": c_dve, "pool": c_pool, "act": c_act}[eng](out)

        def c_pe(lhsT, rhs):
            n = fsz(rhs)
            per = 0.00045 if rhs.dtype == BF16 else 0.00118
            return max(0.11, 0.03 + per * n)

        def dma(eng, out, in_, r=(), w=(), dsem=None, slow=False):
            if dsem is None:
                dsem = "k_" + (w[0] if w else r[0])
            nb_ = 4
            for s in in_.shape:
                nb_ *= s
            lat = 2.0 + nb_ / 180e3
            if slow:
                return P.op("pool", lambda e: e.dma_start(out=out, in_=in_, allow_slow_non_contiguous=True), r=r, w=w, dsem=dsem,
                            cost=0.8, lat=lat + 4.0)
            return P.op(eng, lambda e: e.dma_start(out=out, in_=in_), r=r, w=w, dsem=dsem, cost=0.8 if eng == "pool" else 0.35, lat=lat)

        def mm(out, lhsT, rhs, start, stop, r, w, tp=None):
            if tp is None:
                return P.op("pe", lambda e: e.matmul(out, lhsT=lhsT, rhs=rhs, start=start, stop=stop), r=r, w=w, cost=c_pe(lhsT, rhs))
            return P.op("pe", lambda e: e.matmul(out, lhsT=lhsT, rhs=rhs, start=start, stop=stop, tile_position=tp), r=r, w=w,
                        cost=c_pe(lhsT, rhs))

        def tr(out, in_, ident, r, w):
            return P.op("pe", lambda e: e.transpose(out=out, in_=in_, identity=ident), r=r, w=w, cost=0.2)

        def act(out, in_, func, r, w, bias=None, scale=None, accum=None):
            kw = {}
            if bias is not None:
                kw["bias"] = bias
            if scale is not None:
                kw["scale"] = scale
            if accum is not None:
                kw["accum_out"] = accum
            return P.op("act", lambda e: e.activation(out=out, in_=in_, func=func, **kw), r=r, w=w, cost=c_act(out), grp=ACT_GRP.get(func))

        def tt(eng, out, in0, in1, op, r, w):
            return P.op(eng, lambda e: e.tensor_tensor(out=out, in0=in0, in1=in1, op=op), r=r, w=w, cost=c_eng(eng, out))

        def ts(eng, out, in0, s1, op0, r, w, s2=None, op1=None):
            if op1 is None:
                return P.op(eng, lambda e: e.tensor_scalar(out=out, in0=in0, scalar1=s1, scalar2=None, op0=op0), r=r, w=w, cost=c_eng(eng, out))
            return P.op(eng, lambda e: e.tensor_scalar(out=out, in0=in0, scalar1=s1, scalar2=s2, op0=op0, op1=op1), r=r, w=w,
                        cost=c_eng(eng, out))

        def stt(eng, out, in0, scalar, in1, op0, op1, r, w):
            return P.op(eng, lambda e: e.scalar_tensor_tensor(out=out, in0=in0, scalar=scalar, in1=in1, op0=op0, op1=op1), r=r, w=w,
                        cost=c_eng(eng, out))

        def cp(eng, out, in_, r, w):
            if eng == "act":
                return P.op("act", lambda e: e.copy(out=out, in_=in_), r=r, w=w, cost=c_act(out))
            return P.op(eng, lambda e: e.tensor_copy(out=out, in_=in_), r=r, w=w, cost=c_eng(eng, out))

        def memset(eng, ap, val, w):
            return P.op(eng, lambda e: e.memset(ap, val), w=w, cost=c_eng(eng, ap))

        def dump(name, ap, key, shape):
            if name not in dbg:
                return
            o = nc.dram_tensor("dbg_" + name, list(shape), F32, kind="ExternalOutput").ap()
            dbg_out[name] = o
            dma("sp", o, ap, r=[key], dsem="dbgst")

        bank_ctr = [0]

        def nb():
            b_ = bank_ctr[0] % 8
            bank_ctr[0] += 1
            return b_

        def b3(ap, h=4):
            return ap.rearrange("p (h n) -> p h n", h=h)

        def bc_free(ap, n):
            return ap.unsqueeze(2).broadcast_to([128, ap.shape[1], n])

        stg_ctr = [0]

        def load_T(dst, src, n, key, evac="act"):
            i_ = stg_ctr[0] % 2
            stg_ctr[0] += 1
            stg = stg_slots[i_]
            dma("sp", stg[0:n, :], src, w=[f"stg{i_}"], dsem=f"stg{i_}")
            bk = nb()
            mm(PS[bk][:, 0:n], stg[0:n, :], identf[0:n, 0:n], True, True, [f"stg{i_}", "identf"], [PSK[bk]])
            cp(evac, dst, PS[bk][:, 0:n], [PSK[bk]], [key])

        identf = A.f32(128)
        identb = A.bf16(128)
        ones = A.f32(128)
        eps_t = A.f32(1)
        flag = A.f32(1)
        one_t = A.f32(1)
        memset("pool", identf, 1.0, ["identf"])
        P.op("pool", lambda e: e.affine_select(out=identf, in_=identf, pattern=[[-1, 128]], compare_op=ALU.is_equal,
                                               fill=0.0, base=0, channel_multiplier=1), r=["identf"], w=["identf"])
        cp("dve", identb, identf, ["identf"], ["identb"])
        memset("dve", ones, 1.0, ["ones"])
        memset("dve", eps_t, EPS, ["eps"])
        memset("dve", one_t, 1.0, ["one"])
        dma("sp", flag, flag_d, w=["flag"])
        stg_slots = [A.f32(128), A.f32(128)]
        modT = A.f32(48)
        geff1 = A.f32(D)
        sh1B = A.f32(D)
        consts_mark = A.off

        def sin_of(eng, out, th, tmpf, tmpi, shift, r, w):
            ts(eng, tmpf, th, shift, ALU.add, r, ["_sin_tf"], s2=1.0 / TWO_PI, op1=ALU.mult)
            cp(eng, tmpi, tmpf, ["_sin_tf"], ["_sin_ti"])
            cp(eng, tmpf, tmpi, ["_sin_ti"], ["_sin_tf"])
            ts(eng, tmpf, tmpf, -TWO_PI, ALU.mult, ["_sin_tf"], ["_sin_tf"])
            stt(eng, tmpf, th, shift, tmpf, ALU.add, ALU.add, r + ["_sin_tf"], ["_sin_tf"])
            ts(eng, tmpf, tmpf, -PI_LO, ALU.max, ["_sin_tf"], ["_sin_tf"], s2=PI_LO, op1=ALU.min)
            act(out, tmpf, AF.Sin, ["_sin_tf"], w)

        def bcast_row(dst_fn, col_src, key_src, nt=8):
            for half in range(nt // 4):
                bank = half % 2
                for t4 in range(4):
                    t = half * 4 + t4
                    dg = diag_slots[t % 2]
                    ts("dve", dg, identf, col_src[:, t:t + 1], ALU.mult, ["identf", key_src], [f"diag{t % 2}"])
                    mm(PS[bank][:, t4 * 128:(t4 + 1) * 128], ones, dg, True, True, ["ones", f"diag{t % 2}"], [PSK[bank]])
                dst_fn(half, PS[bank], PSK[bank])

        yT_mark = A.off
        yT = A.bf16(8 * NOWN).rearrange("p (k n) -> p k n", k=8)
        after_yT = A.off

        if "0" in phases:
            A0 = Arena(arena_t[:, yT_mark:after_yT], after_yT - yT_mark)
            cT = A0.f32(8)
            scb = A0.bf16(8)
            badaT = A0.f32(48)
            g1B = A0.f32(D)
            diag_slots = [A0.f32(128), A0.f32(128)]
            wada = [A0.bf16(6144), A0.bf16(6144)]
            load_T(cT, cb.rearrange("o (k p) -> (o k) p", p=128), 8, "cT")
            load_T(badaT, b_ada.rearrange("(m p) -> m p", p=128), 48, "badaT")
            dma("sp", g1B, norm1_g.partition_broadcast(128), w=["g1B"])
            act(scb, cT, AF.Silu, ["cT"], ["scb"])
            for cbk in range(8):
                sl = cbk % 2
                wv = wada[sl].rearrange("p (k n) -> p k n", k=8)
                for k in range(8):
                    dma("pool", wv[:, k, :], w_ada[k * 128:(k + 1) * 128, cbk * 768:(cbk + 1) * 768], w=[f"wada{sl}"], dsem=f"wada{sl}")
                for m6 in range(6):
                    m = cbk * 6 + m6
                    for k in range(8):
                        mm(PS[7][:, m:m + 1], wv[:, k, m6 * 128:(m6 + 1) * 128], scb[:, k:k + 1], k == 0, k == 7,
                           [f"wada{sl}", "scb"], [PSK[7]])
            tt("dve", modT, PS[7][:, 0:48], badaT, ALU.add, [PSK[7], "badaT"], ["modT"])
            dump("modT", modT, "modT", [128, 48])
            bcast_row(lambda h, ps, pk: stt("dve", geff1[:, h * 512:(h + 1) * 512], ps, 1.0, g1B[:, h * 512:(h + 1) * 512],
                                            ALU.add, ALU.mult, [pk, "g1B"], ["geff1"]), modT[:, 8:16], "modT")
            bcast_row(lambda h, ps, pk: cp("act", sh1B[:, h * 512:(h + 1) * 512], ps, [pk], ["sh1B"]), modT[:, 0:8], "modT")
            if "S" not in phases:
                P.barrier()

        def front_tile(t, xt, hb, tmp, hT_view, col, hkey, ss, rstd, junk, fbank=6, evac="act", addeng="pool"):
            sl = t % len(xt) if xt[0] is not xt[1] else 0
            if isinstance(hb, list):
                d2 = t % 2
                hb, tmp, ss, rstd = hb[d2], tmp[d2], ss[d2], rstd[d2]
                junk = tmp
                kh, kt_, ks, kr = f"hb{d2}", f"tmp{d2}", f"ss{d2}", f"rstd{d2}"
            else:
                kh, kt_, ks, kr = "hb", "tmp", "ss", "rstd"
            dma("sp", xt[sl], xs[t * 128:(t + 1) * 128, :], w=[f"xt{sl}"], dsem=f"xt{sl}")
            act(junk, xt[sl], AF.Square, [f"xt{sl}"], [kt_, ks], accum=ss)
            act(rstd, ss, AF.Ln, [ks, "eps"], [kr], bias=eps_t, scale=1.0 / D)
            act(rstd, rstd, AF.Exp, [kr], [kr], scale=-0.5)
            stt("dve", tmp, xt[sl], rstd, geff1, ALU.mult, ALU.mult, [f"xt{sl}", kr, "geff1"], [kt_])
            tt(addeng, hb, tmp, sh1B, ALU.add, [kt_, "sh1B"], [kh])
            pb = PS[fbank].bitcast(BF16)
            for k in range(8):
                tr(pb[:, k * 128:(k + 1) * 128], hb[:, k * 128:(k + 1) * 128], identb, [kh, "identb"], [PSK[fbank]])
            cp(evac, hT_view[:, :, col:col + 128], pb.rearrange("p (k n) -> p k n", k=8), [PSK[fbank]], [hkey])

        if "S" in phases:
            mS = A.off
            Wl = A.bf16(4 * 2 * 8 * 128).rearrange("p (f r j c) -> p f r j c", f=4, r=2, j=8)
            Cw = A.bf16(16 * 2 * 32).rearrange("p (g r c) -> p g r c", g=16, r=2)
            Cs = A.bf16(16 * 2 * 8 * 32).rearrange("p (g r s c) -> p g r s c", g=16, r=2, s=8)
            r8S = A.f32(16); phiS = A.f32(16); phi32S = A.f32(16)
            Ia = A.f32(512); Ib = A.f32(512)
            quarter = A.f32(1); halfpi = A.f32(1)

            uT = A.bf16(4 * NTOK).rearrange("p (f n) -> p f n", f=4)
            dskip = A.f32(4)
            bglu = A.f32(4)
            mP1 = A.off
            xt0_ = A.f32(D)
            xt = [xt0_, xt0_]
            hb = A.bf16(D)
            tmp = A.f32(D)
            junk = tmp
            ss = A.f32(1)
            rstd = A.f32(1)
            hT0_ = A.bf16(8 * 512).rearrange("p (k n) -> p k n", k=8)
            hT = [hT0_, hT0_]
            winu = A.bf16(8 * 512).rearrange("p (k n) -> p k n", k=8)
            mP1end = A.off
            for k in range(8):
                dma("pool", winu[:, k, :], w_in[k * 128:(k + 1) * 128, 0:512], w=["winu"], dsem="winu")
            load_T(dskip, s5_d.rearrange("(f p) -> f p", p=128), 4, "dskip")
            load_T(bglu, b_glu.rearrange("(f p) -> f p", p=128), 4, "bglu")
            mSetup = A.off
            def s5_setup_gen():

                def coef(lr, li, ls, n, key):
                    ar = A.f32(n); ai = A.f32(n); cfr = A.f32(n); cfi = A.f32(n)
                    m1 = A.off
                    step = A.f32(n); mag = A.f32(n); th = A.f32(n); tf = A.f32(n); ti = A.i32(n); sn = A.f32(n); cs = A.f32(n)
                    t1 = A.f32(n); t2 = A.f32(n); den = A.f32(n)
                    k = lambda s: f"{key}_{s}"
                    act(step, ls, AF.Exp, [k("ls")], [k("step")])
                    tt("dve", mag, lr, step, ALU.mult, [k("lr"), k("step")], [k("mag")])
                    act(mag, mag, AF.Exp, [k("mag")], [k("mag")])
                    tt("dve", th, li, step, ALU.mult, [k("li"), k("step")], [k("th")])
                    sin_of("dve", sn, th, tf, ti, 0.0, [k("th")], [k("sn")])
                    sin_of("dve", cs, th, tf, ti, math.pi / 2, [k("th")], [k("cs")])
                    tt("dve", ar, mag, cs, ALU.mult, [k("mag"), k("cs")], [k("ar")])
                    tt("dve", ai, mag, sn, ALU.mult, [k("mag"), k("sn")], [k("ai")])
                    ts("dve", t1, ar, -1.0, ALU.add, [k("ar")], [k("t1")])
                    tt("dve", den, lr, lr, ALU.mult, [k("lr")], [k("den")])
                    tt("dve", t2, li, li, ALU.mult, [k("li")], [k("t2")])
                    tt("dve", den, den, t2, ALU.add, [k("den"), k("t2")], [k("den")])
                    P.op("dve", lambda e: e.reciprocal(out=den, in_=den), r=[k("den")], w=[k("den")])
                    tt("dve", cfr, t1, lr, ALU.mult, [k("t1"), k("lr")], [k("cfr")])
                    tt("dve", t2, ai, li, ALU.mult, [k("ai"), k("li")], [k("t2")])
                    tt("dve", cfr, cfr, t2, ALU.add, [k("cfr"), k("t2")], [k("cfr")])
                    tt("dve", cfr, cfr, den, ALU.mult, [k("cfr"), k("den")], [k("cfr")])
                    tt("dve", cfi, ai, lr, ALU.mult, [k("ai"), k("lr")], [k("cfi")])
                    tt("dve", t2, t1, li, ALU.mult, [k("t1"), k("li")], [k("t2")])
                    tt("dve", cfi, cfi, t2, ALU.subtract, [k("cfi"), k("t2")], [k("cfi")])
                    tt("dve", cfi, cfi, den, ALU.mult, [k("cfi"), k("den")], [k("cfi")])
                    return ar, ai, cfr, cfi

                def cmul(outr, outi, ar_, ai_, br_, bi_, t1, t2, keys_r, key_w, tk1, tk2):
                    tt("dve", outr, ar_, br_, ALU.mult, keys_r, [key_w + "r"])
                    tt("dve", t1, ai_, bi_, ALU.mult, keys_r, [tk1])
                    tt("dve", outr, outr, t1, ALU.subtract, [key_w + "r", tk1], [key_w + "r"])
                    tt("dve", outi, ar_, bi_, ALU.mult, keys_r, [key_w + "i"])
                    tt("dve", t2, ai_, br_, ALU.mult, keys_r, [tk2])
                    tt("dve", outi, outi, t2, ALU.add, [key_w + "i", tk2], [key_w + "i"])

                lrK = A.f32(512); liK = A.f32(512); lsK = A.f32(512); lsSm = A.f32(8)
                for q in range(4):
                    dma("sp", lrK[32 * q:32 * q + 32, :].rearrange("p (f c) -> p f c", f=4), dram_ap(lam_re, q * 128, [[0, 32], [512, 4], [1, 128]]), w=["K_lr"])
                    dma("sp", liK[32 * q:32 * q + 32, :].rearrange("p (f c) -> p f c", f=4), dram_ap(lam_im, q * 128, [[0, 32], [512, 4], [1, 128]]), w=["K_li"])
                    dma("sp", lsSm[32 * q:32 * q + 32, :].rearrange("p (f t) -> p f t", f=4),
                        dram_ap(log_step, q * 2, [[0, 32], [8, 4], [1, 2]]), w=["lsSm"], slow=True)
                cp("dve", lsK.rearrange("p (f t c) -> p f t c", f=4, t=2), lsSm.rearrange("p (f t) -> p f t", f=4).unsqueeze(3).broadcast_to([128, 4, 2, 64]), ["lsSm"], ["K_ls"])
                yield
                arK, aiK, cfrK, cfiK = coef(lrK, liK, lsK, 512, "K")
                yield
                Bre = A.f32(512); Bim = A.f32(512)
                Bst = [A.f32(512), A.f32(512)]
                for ai_, (src, dst, kk) in enumerate(((s5_b_re, Bre, "Bre"), (s5_b_im, Bim, "Bim"))):
                    bst = Bst[ai_]
                    memset("pool", bst, 0.0, [f"Bst{ai_}"])
                    bst4 = bst.rearrange("p (f b h) -> p f b h", f=4, b=8)
                    for two in range(2):
                        for f_ in range(4):
                            dma("sp" if ai_ == 0 else "act", bst4[64 * two:64 * two + 64, f_, two::2, :],
                                dram_ap(src, (8 * f_ + two) * 1024, [[16, 64], [2048, 4], [1, 16]]), w=[f"Bst{ai_}"], dsem=f"bst{ai_}")
                    bk = nb()
                    for f_ in range(4):
                        tr(PS[bk][:, f_ * 128:(f_ + 1) * 128], bst[:, f_ * 128:(f_ + 1) * 128], identf, [f"Bst{ai_}", "identf"], [PSK[bk]])
                    cp("act", dst, PS[bk], [PSK[bk]], [kk])
                Kr = [A.f32(512), A.f32(512)]; Ki = [A.f32(512), A.f32(512)]
                t1K = A.f32(512); t2K = A.f32(512); t3K = A.f32(512)
                cp("dve", Kr[0], cfrK, ["K_cfr"], ["K0r"])
                cp("dve", Ki[0], cfiK, ["K_cfi"], ["K0i"])
                for j in range(8):
                    a = j % 2
                    kr, ki = Kr[a], Ki[a]
                    krk, kik = f"K{a}r", f"K{a}i"
                    tt("dve", t1K, kr, Bre, ALU.mult, [krk, "Bre"], ["t1K"])
                    tt("dve", t2K, ki, Bim, ALU.mult, [kik, "Bim"], ["t2K"])
                    tt("dve", Wl[:, :, 0, j, :], t1K.rearrange("p (f c) -> p f c", f=4), t2K.rearrange("p (f c) -> p f c", f=4), ALU.subtract, ["t1K", "t2K"], ["Wl"])
                    tt("dve", t1K, kr, Bim, ALU.mult, [krk, "Bim"], ["t1K"])
                    tt("dve", t2K, ki, Bre, ALU.mult, [kik, "Bre"], ["t2K"])
                    tt("dve", Wl[:, :, 1, j, :], t1K.rearrange("p (f c) -> p f c", f=4), t2K.rearrange("p (f c) -> p f c", f=4), ALU.add, ["t1K", "t2K"], ["Wl"])
                    if j < 7:
                        b = 1 - a
                        cmul(Kr[b], Ki[b], kr, ki, arK, aiK, t1K, t3K, [krk, kik, "K_ar", "K_ai"], f"K{b}", "t1K", "t3K")
                    yield

                mSL = A.off
                lrS = A.f32(16); liS = A.f32(16); lsS = A.f32(16)
                load_T(lrS, lam_re.rearrange("(g t) p -> g (t p)", t=2), 16, "S_lr")
                load_T(liS, lam_im.rearrange("(g t) p -> g (t p)", t=2), 16, "S_li")
                lsm2 = A.f32(2)
                lsw = A.f32(128)
                dma("sp", lsm2[0:16, :], log_step.rearrange("(g t) -> g t", t=2), w=["lsm2"])
                cp("dve", lsw[0:16, :].rearrange("p (t c) -> p t c", t=2), lsm2[0:16, :].unsqueeze(2).broadcast_to([16, 2, 64]), ["lsm2"], ["lsw"])
                bk = nb()
                mm(PS[bk][:, 0:16], lsw[0:16, :], identf[0:16, 0:16], True, True, ["lsw", "identf"], [PSK[bk]])
                cp("act", lsS, PS[bk][:, 0:16], [PSK[bk]], ["S_ls"])
                yield
                arS, aiS, _, _ = coef(lrS, liS, lsS, 16, "S")
                yield
                PWre = A.f32(9 * 16).rearrange("p (k g) -> p k g", k=9)
                PWim = A.f32(9 * 16).rearrange("p (k g) -> p k g", k=9)
                t1S = A.f32(16); t2S = A.f32(16)
                memset("dve", PWre[:, 0, :], 1.0, ["PW0r"])
                memset("dve", PWim[:, 0, :], 0.0, ["PW0i"])
                cp("dve", PWre[:, 1, :], arS, ["S_ar"], ["PW1r"])
                cp("dve", PWim[:, 1, :], aiS, ["S_ai"], ["PW1i"])
                for j in range(2, 9):
                    cmul(PWre[:, j, :], PWim[:, j, :], PWre[:, j - 1, :], PWim[:, j - 1, :], arS, aiS, t1S, t2S,
                         [f"PW{j - 1}r", f"PW{j - 1}i", "S_ar", "S_ai"], f"PW{j}", "t1S", "t2S")
                st8 = A.f32(16); tS_a = A.f32(16); t32 = A.f32(16); tfS = A.f32(16); tiS = A.i32(16)
                act(st8, lsS, AF.Exp, ["S_ls"], ["st8"])
                tt("dve", tS_a, lrS, st8, ALU.mult, ["S_lr", "st8"], ["tS_a"])
                act(r8S, tS_a, AF.Exp, ["tS_a"], ["r8S"], scale=8.0)
                tt("dve", phiS, liS, st8, ALU.mult, ["S_li", "st8"], ["phiS"])
                ts("dve", phiS, phiS, 8.0, ALU.mult, ["phiS"], ["phiS"])
                ts("dve", t32, phiS, 32.0, ALU.mult, ["phiS"], ["t32"])
                ts("dve", tfS, t32, 1.0 / TWO_PI, ALU.mult, ["t32"], ["tfS"])
                cp("dve", tiS, tfS, ["tfS"], ["tiS"])
                cp("dve", tfS, tiS, ["tiS"], ["tfS"])
                stt("dve", phi32S, tfS, -TWO_PI, t32, ALU.mult, ALU.add, ["tfS", "t32"], ["phi32S"])
                ones512 = t3K; cidx = A.f32(512); tiI = A.i32(512)
                memset("dve", ones512, 1.0, ["t3K"])
                memset("dve", quarter, 0.25, ["quarter"])
                memset("dve", halfpi, math.pi / 2, ["halfpi"])
                P.op("dve", lambda e: e.tensor_tensor_scan(out=cidx, data0=ones512, data1=ones512, initial=-1.0, op0=ALU.mult, op1=ALU.add),
                     r=["t3K"], w=["cidx"], cost=1.2)
                ts("dve", Ia, cidx, -15.5, ALU.add, ["cidx"], ["Ia"], s2=1.0 / 32, op1=ALU.mult)
                cp("dve", tiI, Ia, ["Ia"], ["tiI"])
                cp("dve", Ia, tiI, ["tiI"], ["Ia"])
                stt("dve", Ib, Ia, -32.0, cidx, ALU.mult, ALU.add, ["Ia", "cidx"], ["Ib"])
                C0re = A.f32(16 * 32); C0im = A.f32(16 * 32)
                for ai_, (src, dst, kk) in enumerate(((s5_c_re, C0re, "C0re"), (s5_c_im, C0im, "C0im"))):
                    cst = Bst[ai_]
                    memset("pool", cst, 0.0, [f"Bst{ai_}"])
                    cst3 = cst.rearrange("p (j c) -> p j c", j=4)
                    for b8 in range(8):
                        dma("sp" if ai_ == 0 else "act", cst3[16 * b8:16 * b8 + 16, :, 64 * (b8 % 2):64 * (b8 % 2) + 64],
                            dram_ap(src, b8 * 1024, [[64, 16], [8192, 4], [1, 64]]), w=[f"Bst{ai_}"], dsem=f"bst{ai_}")
                    bk = nb()
                    for j_ in range(4):
                        tr(PS[bk][:, j_ * 128:(j_ + 1) * 128], cst[:, j_ * 128:(j_ + 1) * 128], identf, [f"Bst{ai_}", "identf"], [PSK[bk]])
                    cp("act", dst, PS[bk], [PSK[bk]], [kk])
                C0re3 = C0re.rearrange("p (g c) -> p g c", g=16)
                C0im3 = C0im.rearrange("p (g c) -> p g c", g=16)
                cp("dve", Cw[:, :, 0, :], C0re3, ["C0re"], ["Cw"])
                ts("dve", Cw[:, :, 1, :], C0im3, -1.0, ALU.mult, ["C0im"], ["Cw"])
                tA = t1K; tB = t2K
                tA3 = tA.rearrange("p (g c) -> p g c", g=16); tB3 = tB.rearrange("p (g c) -> p g c", g=16)
                for s in range(8):
                    a_bc = PWre[:, s + 1, :].unsqueeze(2).broadcast_to([128, 16, 32])
                    b_bc = PWim[:, s + 1, :].unsqueeze(2).broadcast_to([128, 16, 32])
                    kr_ = [f"PW{s + 1}r", f"PW{s + 1}i", "C0re", "C0im"]
                    tt("dve", tA3, C0re3, a_bc, ALU.mult, kr_, ["t1K"])
                    tt("dve", tB3, C0im3, b_bc, ALU.mult, kr_, ["t2K"])
                    tt("dve", Cs[:, :, 0, s, :], tA3, tB3, ALU.subtract, ["t1K", "t2K"], ["Cs"])
                    tt("dve", tA3, C0re3, b_bc, ALU.mult, kr_, ["t1K"])
                    tt("dve", tB3, C0im3, a_bc, ALU.mult, kr_, ["t2K"])
                    tt("dve", tA3, tA3, tB3, ALU.add, ["t1K", "t2K"], ["t1K"])
                    ts("dve", Cs[:, :, 1, s, :], tA3, -1.0, ALU.mult, ["t1K"], ["Cs"])
                    yield
                yield
            def s5_pass1_gen():
                for grp in range(8 if int(os.environ.get("SSTOP", "9")) >= 1 else 0):
                    hs = grp % 2
                    for t4 in range(4):
                        front_tile(grp * 4 + t4, xt, hb, tmp, hT[hs], t4 * 128, f"hT{hs}", ss, rstd, junk, fbank=6 + (grp * 4 + t4) % 2, evac="dve", addeng="dve")
                        yield
                    for f in range(4):
                        bank = f % 4
                        for k in range(8):
                            mm(PS[bank], winu[:, k, f * 128:(f + 1) * 128], hT[hs][:, k, :], k == 0, k == 7, ["winu", f"hT{hs}"], [PSK[bank]])
                        dst = uT[:, f, grp * 512:(grp + 1) * 512]
                        if grp < 4:
                            act(dst, PS[bank], AF.Copy, [PSK[bank], "flag"], ["uT"], scale=flag)
                        else:
                            cp("act", dst, PS[bank], [PSK[bank]], ["uT"])
                        yield


                yield


            for _ in s5_setup_gen():
                pass
            P.barrier("S_setup")
            A.off = mSetup
            xt = [xt0_, A.f32(D), A.f32(D), A.f32(D)]
            hb = [hb, A.bf16(D)]
            tmp = [tmp, A.f32(D)]
            ss = [ss, A.f32(1)]
            rstd = [rstd, A.f32(1)]
            hT = [hT0_, A.bf16(8 * 512).rearrange("p (k n) -> p k n", k=8)]
            for _ in s5_pass1_gen():
                pass
            P.barrier("S_p1")
            A.off = mP1
            Xprev = A.bf16(16 * 2 * 256).rearrange("p (g r n) -> p g r n", g=16, r=2)
            wglu = A.bf16(4 * 512).rearrange("p (k n) -> p k n", k=4)
            for k in range(4):
                dma("pool", wglu[:, k, :], w_glu[k * 128:(k + 1) * 128, :], w=["wglu"], dsem="wglu")
            mP1 = A.off

            sstop = int(os.environ.get("SSTOP", "9"))
            _sb = [0]

            def nb_s():
                _sb[0] += 1
                return 6 + _sb[0] % 2
            A.off = mP1
            mG = A.off
            Gs = [[A.f32(512), A.f32(512)] for _ in range(4)]
            ang = A.f32(512); angb = A.f32(512)
            tfA = A.f32(512); tiA = A.i32(512); tfB = A.f32(512); tiB = A.i32(512)
            cosT = A.f32(512); sinT = A.f32(512)
            p1 = A.f32(512); p2 = A.f32(512); p3 = A.f32(512); p4 = A.f32(512)
            wre = A.f32(512); wim = A.f32(512); zre = A.f32(512); zim = A.f32(512)
            r8t = A.f32(512)
            AY = Arena(arena_t[:, yT_mark + 4096:after_yT], 4096)
            dbl = {"cosT": [cosT, AY.f32(512)], "sinT": [sinT, AY.f32(512)], "p1": [p1, AY.f32(512)], "p2": [p2, AY.f32(512)],
                   "p3": [p3, AY.f32(512)], "p4": [p4, AY.f32(512)], "wre": [wre, AY.f32(512)], "wim": [wim, AY.f32(512)]}

            def mm_bundle(specs, r, w, cost):
                def fn(e):
                    ins = None
                    for (o_, l_, r_, st_, sp_, tp_) in specs:
                        ins = e.matmul(o_, lhsT=l_, rhs=r_, start=st_, stop=sp_, tile_position=tp_)
                    return ins
                return P.op("pe", fn, r=r, w=w, cost=cost)

            for f in range(4 if sstop >= 2 else 0):
                for ri in range(2):
                    banks = [4 * ri + q for q in range(4)]
                    for s_ in range(8):
                        specs = [(PS[banks[q]], Wl[32 * q:32 * q + 32, f, ri, 7 - s_, :], uT[32 * q:32 * q + 32, f, s_::8], s_ == 0, s_ == 7, (32 * q, 0))
                                 for q in range(4)]
                        mm_bundle(specs, ["Wl", "uT"], [PSK[b] for b in banks], 0.95)
                    for q in range(4):
                        cp("act", Gs[q][ri], PS[banks[q]], [PSK[banks[q]]], [f"G{q}{ri}"])
                for q in range(4):
                    gp = f * 4 + q
                    Gre, Gim = Gs[q]
                    gk = [f"G{q}0", f"G{q}1"]
                    pb_ = gp % 2
                    cosT, sinT, p1, p2, p3, p4, wre, wim = (dbl[n_][pb_] for n_ in ("cosT", "sinT", "p1", "p2", "p3", "p4", "wre", "wim"))
                    kC, kS, k1, k2, k3, k4, kwr, kwi = (f"{n_}{pb_}" for n_ in ("cosT", "sinT", "p1", "p2", "p3", "p4", "wre", "wim"))
                    act(angb, Ib, AF.Copy, ["Ib", "phiS"], ["angb"], scale=phiS[:, gp:gp + 1])
                    stt("dve", ang, Ia, phi32S[:, gp:gp + 1], angb, ALU.mult, ALU.add, ["Ia", "phi32S", "angb"], ["ang"])
                    act(r8t, Ia, AF.Identity, ["Ia", "r8S"], ["r8t"], scale=0.0, bias=r8S[:, gp:gp + 1])
                    act(tfA, ang, AF.Copy, ["ang"], ["tfA"], scale=1.0 / TWO_PI)
                    cp("dve", tiA, tfA, ["tfA"], ["tiA"])
                    cp("dve", tfA, tiA, ["tiA"], ["tfA"])
                    stt("dve", tfA, tfA, -TWO_PI, ang, ALU.mult, ALU.add, ["tfA", "ang"], ["tfA"])
                    ts("dve", tfA, tfA, -PI_LO, ALU.max, ["tfA"], ["tfA"], s2=PI_LO, op1=ALU.min)
                    act(sinT, tfA, AF.Sin, ["tfA"], [kS])
                    act(tfB, tfA, AF.Abs, ["tfA"], ["tfB"])
                    act(cosT, tfB, AF.Sin, ["tfB", "halfpi"], [kC], bias=halfpi, scale=-1.0)
                    tt("pool", p1, Gre, cosT, ALU.mult, gk + [kC], [k1])
                    tt("pool", p2, Gim, sinT, ALU.mult, gk + [kS], [k2])
                    tt("pool", p3, Gim, cosT, ALU.mult, gk + [kC], [k3])
                    tt("pool", p4, Gre, sinT, ALU.mult, gk + [kS], [k4])
                    tt("dve", wre, p1, p2, ALU.add, [k1, k2], [kwr])
                    tt("dve", wim, p3, p4, ALU.subtract, [k3, k4], [kwi])
                    P.op("dve", lambda e, wre=wre: e.tensor_tensor_scan(out=zre, data0=r8t, data1=wre, initial=0.0, op0=ALU.mult, op1=ALU.add),
                         r=["r8t", kwr], w=["zre"], cost=1.2)
                    P.op("dve", lambda e, wim=wim: e.tensor_tensor_scan(out=zim, data0=r8t, data1=wim, initial=0.0, op0=ALU.mult, op1=ALU.add),
                         r=["r8t", kwi], w=["zim"], cost=1.2)
                    cs_ = slice(255, 511)
                    tt("pool", p1[:, cs_], zre[:, cs_], cosT[:, cs_], ALU.mult, ["zre", kC], [k1])
                    tt("pool", p2[:, cs_], zim[:, cs_], sinT[:, cs_], ALU.mult, ["zim", kS], [k2])
                    tt("pool", p3[:, cs_], zre[:, cs_], sinT[:, cs_], ALU.mult, ["zre", kS], [k3])
                    tt("pool", p4[:, cs_], zim[:, cs_], cosT[:, cs_], ALU.mult, ["zim", kC], [k4])
                    tt("dve", Xprev[:, gp, 0, :], p1[:, cs_], p2[:, cs_], ALU.subtract, [k1, k2], [f"Xprev{f}"])
                    tt("dve", Xprev[:, gp, 1, :], p3[:, cs_], p4[:, cs_], ALU.add, [k3, k4], [f"Xprev{f}"])

            xtb = [[A.bf16(512) for _ in range(2)] for _ in range(4)]
            yg = A.f32(4 * 512).rearrange("p (f n) -> p f n", f=4)
            ygb = A.bf16(4 * 512).rearrange("p (f n) -> p f n", f=4)
            sig = A.f32(512)
            for ct in range(4 if sstop >= 3 else 0):
                tok0 = NOWN + ct * 512
                for f in range(4):
                    ybank = f % 2
                    yps = PS[ybank]
                    ypsv = yps.rearrange("p (c s) -> p c s", s=8)
                    for ri in range(2):
                        banks = [2 + q for q in range(4)]
                        for j in range(8):
                            specs = []
                            for q in range(4):
                                xv = PS[banks[q]].rearrange("p (c s) -> p c s", s=8)
                                uv = uT[32 * q:32 * q + 32, f, tok0:tok0 + 512].rearrange("p (c s) -> p c s", s=8)
                                specs.append((xv[:, :, j:8], Wl[32 * q:32 * q + 32, f, ri, j, :], uv[:, :, 0:8 - j], j == 0, j == 7, (32 * q, 0)))
                            mm_bundle(specs, ["Wl", "uT"], [PSK[b] for b in banks], 0.6)
                        for q in range(4):
                            cp("act" if q % 2 == 0 else "dve", xtb[q][ri], PS[banks[q]], [PSK[banks[q]]], [f"xtb{q}{ri}"])
                        specs = [(yps[32 * q:32 * q + 32, :], Cw[:, f * 4 + q, ri, :], xtb[q][ri], ri == 0, False, (0, 32 * q)) for q in range(4)]
                        mm_bundle(specs, ["Cw"] + [f"xtb{q}{ri}" for q in range(4)], [PSK[ybank]], 0.5)
                    for s in range(8):
                        for ri in range(2):
                            specs = [(ypsv[32 * q:32 * q + 32, :, s], Cs[:, f * 4 + q, ri, s, :], Xprev[:, f * 4 + q, ri, ct * 64:(ct + 1) * 64],
                                      False, (s == 7 and ri == 1), (0, 32 * q)) for q in range(4)]
                            mm_bundle(specs, ["Cs", f"Xprev{f}"], [PSK[ybank]], 0.3)
                    stt("dve", yg[:, f, :], uT[:, f, tok0:tok0 + 512], dskip[:, f:f + 1], yps, ALU.mult, ALU.add, ["uT", "dskip", PSK[ybank]], ["yg"])
                    act(yg[:, f, :], yg[:, f, :], AF.Gelu_apprx_tanh, ["yg"], ["yg"])
                    cp("pool", ygb[:, f, :], yg[:, f, :], ["yg"], ["ygb"])
                for mt in range(4):
                    bank = 6 + mt % 2
                    for kt in range(4):
                        mm(PS[bank], wglu[:, kt, mt * 128:(mt + 1) * 128], ygb[:, kt, :], kt == 0, kt == 3, ["wglu", "ygb"], [PSK[bank]])
                    act(sig, PS[bank], AF.Sigmoid, [PSK[bank], "bglu"], ["sig"], bias=bglu[:, mt:mt + 1])
                    tt("dve", yT[:, mt, ct * 512:(ct + 1) * 512], yg[:, mt, :], sig, ALU.mult, ["yg", "sig"], ["yT"])
            P.barrier("S_p23")
            A.off = mS


        if "G" in phases:
            mGd = A.off
            xt = [A.f32(D), A.f32(D)]
            hb = A.bf16(D)
            tmp = A.f32(D)
            junk = tmp
            ss = A.f32(1)
            rstd = A.f32(1)
            hT1 = [A.bf16(8 * 128).rearrange("p (k n) -> p k n", k=8) for _ in range(2)]
            Wr = A.bf16(8 * 2056).rearrange("p (k n) -> p k n", k=8)
            pre = A.f32(12 * 131).rearrange("p (f n) -> p f n", f=12)
            cv = A.f32(12 * 128).rearrange("p (f n) -> p f n", f=12)
            qkv = A.f32(8 * 128)
            sq = A.f32(8 * 128)
            rn = A.f32(8 * 128)
            qn = A.f32(512); kn = A.f32(512)
            cwT = A.f32(48)
            ab = A.f32(8)
            zs = A.f32(512)
            gsp = A.f32(4); g_ = A.f32(4); beta = A.f32(4); gmask = A.f32(8); gcl = A.f32(16)
            eg = A.f32(4); ktw = A.f32(4); eglB = A.f32(8); bkeg = A.f32(4)
            dtb = A.f32(4); nae = A.f32(4); gngB = A.f32(128); mA = A.f32(1); mB = A.f32(1)
            Tri = A.f32(128); Bones = A.f32(128); Mst = A.f32(128); Min = A.f32(128)
            M4s = A.f32(512); M4i = A.f32(512)
            diagG = A.f32(512); dmx = A.f32(512); E_ = A.f32(512); EL = A.f32(512); EQ = A.f32(512)
            L_ = A.f32(512); QK = A.f32(512); QKT = A.f32(512)
            Nb = [A.f32(512), A.f32(512)]; Mb = [A.f32(512), A.f32(512)]; Xb = [A.f32(512), A.f32(512)]
            vb = A.f32(512); kbg = A.f32(512); kt = A.f32(512); value = A.f32(512); kcT = A.f32(512)
            vnew = A.f32(512); S_ = A.f32(512); Stmp = A.f32(512); o_t = A.f32(512); otmp = A.f32(512)
            oss = A.f32(4); orstd = A.f32(4); ygd = A.bf16(512)
            tmpP = A.f32(128)

            for k in range(8):
                dma("pool", Wr[:, k, :], w_in[k * 128:(k + 1) * 128, 512:2568], w=["Wr"], dsem="Wr")
            load_T(cwT, conv_w.rearrange("j (f p) -> (j f) p", p=128), 48, "cwT")
            dma("sp", dtb, dt_bias.partition_broadcast(128), w=["dtb"])
            dma("sp", nae, a_log.partition_broadcast(128), w=["nae"])
            dma("sp", gngB, gdn_norm_g.partition_broadcast(128), w=["gngB"])
            act(nae, nae, AF.Exp, ["nae"], ["nae"])
            ts("dve", nae, nae, -1.0, ALU.mult, ["nae"], ["nae"])
            memset("dve", mA, 0.0, ["mA"]); memset("dve", mB, 0.0, ["mB"])
            memset("dve", mA[0:64, :], 1.0, ["mA"]); memset("dve", mB[64:128, :], 1.0, ["mB"])
            memset("pool", Bones, 0.0, ["Bones"])
            memset("pool", Bones[0:64, 0:64], 1.0, ["Bones"])
            memset("pool", Bones[64:128, 64:128], 1.0, ["Bones"])

            def sel(dst, pattern, cm, cmp_op, key):
                cp("pool", dst, Bones, ["Bones"], [key])
                P.op("pool", lambda e: e.affine_select(out=dst, in_=dst, pattern=pattern, compare_op=cmp_op, fill=0.0, base=0,
                                                       channel_multiplier=cm), r=[key], w=[key])
            sel(Tri, [[1, 128]], -1, ALU.is_ge, "Tri")
            sel(Mst, [[-1, 128]], 1, ALU.is_gt, "Mst")
            sel(Min, [[-1, 128]], 1, ALU.is_ge, "Min")
            cp("pool", b3(M4s), Mst.unsqueeze(1).broadcast_to([128, 4, 128]), ["Mst"], ["M4s"])
            cp("pool", b3(M4i), Min.unsqueeze(1).broadcast_to([128, 4, 128]), ["Min"], ["M4i"])
            memset("dve", pre, 0.0, ["pre"])
            memset("dve", S_, 0.0, ["S"])
            ident4 = identf.unsqueeze(1).broadcast_to([128, 4, 128])

            qnS = [qn, A.f32(512), A.f32(512)]
            knS = [kn, A.f32(512), A.f32(512)]
            vvS = [A.f32(512) for _ in range(3)]
            zsS = [zs, A.f32(512), A.f32(512)]
            gsS = [A.f32(48) for _ in range(3)]
            QKTS = [QKT, A.f32(512)]
            ktS = [kt, A.f32(512)]
            valS = [value, A.f32(512)]
            kcTS = [kcT, A.f32(512)]
            qk_ = qkv[:, 0:1024]

            def hs4(h):
                return slice(h * 128, (h + 1) * 128)

            _bc = {"A1": 0, "A2": 0, "B": 0}

            def nbA1():
                _bc["A1"] += 1
                return _bc["A1"] % 2

            def nbA2():
                _bc["A2"] += 1
                return 2 + _bc["A2"] % 3

            def nbB():
                _bc["B"] += 1
                return 5 + _bc["B"] % 3

            def stageA1(t):
                own = t >= 16
                hs = t % 2
                s3 = t % 3
                hk = f"hT1{hs}"
                gs = gsS[s3]
                beta = gs[:, 0:4]; gcl = gs[:, 4:20]; eg = gs[:, 20:24]; ktw = gs[:, 24:28]; eglB = gs[:, 28:36]; bkeg = gs[:, 36:40]
                gsk = f"gs{s3}"
                front_tile(t, xt, hb, tmp, hT1[hs], 0, hk, ss, rstd, junk, fbank=nbA1())
                yield
                for f3 in range(3):
                    bk = nbA1()
                    for f4 in range(4):
                        f = f3 * 4 + f4
                        for k in range(8):
                            mm(PS[bk][:, f4 * 128:(f4 + 1) * 128], Wr[:, k, f * 128:(f + 1) * 128], hT1[hs][:, k, :], k == 0, k == 7, ["Wr", hk], [PSK[bk]])
                    dst = pre[:, f3 * 4:(f3 + 1) * 4, 3:131]
                    if not own:
                        act(dst, b3(PS[bk]), AF.Copy, [PSK[bk], "flag"], ["pre"], scale=flag)
                    else:
                        cp("act", dst, b3(PS[bk]), [PSK[bk]], ["pre"])
                    yield
                bk = nbA1()
                for k in range(8):
                    mm(PS[bk][:, 0:8], hT1[hs][:, k, :], Wr[:, k, 2048:2056], k == 0, k == 7, ["Wr", hk], [PSK[bk]])
                if not own:
                    act(ab, PS[bk][:, 0:8], AF.Copy, [PSK[bk], "flag"], ["ab"], scale=flag)
                else:
                    cp("act", ab, PS[bk][:, 0:8], [PSK[bk]], ["ab"])
                if own:
                    bk = nbA1()
                    for k in range(8):
                        mm(PS[bk], hT1[hs][:, k, :], Wr[:, k, 1536:2048], k == 0, k == 7, ["Wr", hk], [PSK[bk]])
                    act(zsS[s3], PS[bk], AF.Silu, [PSK[bk]], [f"zs{s3}"])
                yield
                for f in range(12):
                    ck = f"cv{f}"
                    act(cv[:, f, :], pre[:, f, 3:131], AF.Copy, ["pre", "cwT"], [ck], scale=cwT[:, 36 + f:36 + f + 1])
                    for j in range(3):
                        stt("dve", cv[:, f, :], pre[:, f, j:j + 128], cwT[:, j * 12 + f:j * 12 + f + 1], cv[:, f, :], ALU.mult, ALU.add, ["pre", "cwT", ck], [ck])
                    if f % 3 == 2:
                        yield
                cp("pool", pre[:, :, 0:3], pre[:, :, 128:131], ["pre"], ["pre"])
                cvf = cv.rearrange("p f n -> p (f n)")
                act(qk_, cvf[:, 0:1024], AF.Silu, [f"cv{f}" for f in range(8)], ["qk"])
                act(vvS[s3], cvf[:, 1024:1536], AF.Silu, [f"cv{f}" for f in range(8, 12)], [f"vv{s3}"])
                yield
                tt("dve", sq, qk_, qk_, ALU.mult, ["qk"], ["sq"])
                for hf in range(2):
                    bk = nbA1()
                    mm(PS[bk], ones, sq[:, hf * 512:(hf + 1) * 512], True, True, ["ones", "sq"], [PSK[bk]])
                    act(rn[:, hf * 512:(hf + 1) * 512], PS[bk], AF.Ln, [PSK[bk], "eps"], ["rn"], bias=eps_t)
                yield
                act(rn, rn, AF.Exp, ["rn"], ["rn"], scale=-0.5)
                stt("dve", qnS[s3], qk_[:, 0:512], 128.0 ** -0.5, rn[:, 0:512], ALU.mult, ALU.mult, ["qk", "rn"], [f"qn{s3}"])
                tt("dve", knS[s3], qk_[:, 512:1024], rn[:, 512:1024], ALU.mult, ["qk", "rn"], [f"kn{s3}"])
                yield
                tt("dve", gsp, ab[:, 0:4], dtb, ALU.add, ["ab", "dtb"], ["gsp"])
                act(gsp, gsp, AF.Exp, ["gsp"], ["gsp"])
                act(gsp, gsp, AF.Ln, ["gsp", "one"], ["gsp"], bias=one_t)
                tt("dve", g_, gsp, nae, ALU.mult, ["gsp", "nae"], ["g"])
                act(beta, ab[:, 4:8], AF.Exp, ["ab"], [gsk], scale=-1.0)
                ts("dve", beta, beta, 1.0, ALU.add, [gsk], [gsk])
                P.op("dve", lambda e, beta=beta: e.reciprocal(out=beta, in_=beta), r=[gsk], w=[gsk])
                yield
                ts("dve", gmask[:, 0:4], g_, mA, ALU.mult, ["g", "mA"], ["gmask"])
                ts("dve", gmask[:, 4:8], g_, mB, ALU.mult, ["g", "mB"], ["gmask"])
                bk = nbA1()
                mm(PS[bk][:, 0:4], Tri, g_, True, True, ["Tri", "g"], [PSK[bk]])
                mm(PS[bk][:, 4:8], Bones, g_, True, True, ["Bones", "g"], [PSK[bk]])
                mm(PS[bk][:, 8:16], ones, gmask, True, True, ["ones", "gmask"], [PSK[bk]])
                cp("dve", gcl, PS[bk][:, 0:16], [PSK[bk]], [gsk])
                yield
                gc = gcl[:, 0:4]; gl = gcl[:, 4:8]; glB = gcl[:, 8:16]
                act(eg, gc, AF.Exp, [gsk], [gsk])
                tt("dve", ktw, gl, gc, ALU.subtract, [gsk], [gsk])
                act(ktw, ktw, AF.Exp, [gsk], [gsk])
                act(eglB, glB, AF.Exp, [gsk], [gsk])
                tt("dve", bkeg, beta, eg, ALU.mult, [gsk], [gsk])
                yield

            def stageA2(t):
                s3 = t % 3
                s2 = t % 2
                gs = gsS[s3]
                beta = gs[:, 0:4]; gcl = gs[:, 4:20]; ktw = gs[:, 24:28]; bkeg = gs[:, 36:40]
                gc = gcl[:, 0:4]
                gsk = f"gs{s3}"
                qn_, kn_, vv_ = qnS[s3], knS[s3], vvS[s3]
                qnk, knk, vvk = f"qn{s3}", f"kn{s3}", f"vv{s3}"
                tt("dve", b3(diagG), ident4, bc_free(gc, 128), ALU.mult, ["identf", gsk], ["diagG"])
                bk = nbA2()
                mm(PS[bk], ones, diagG, True, True, ["ones", "diagG"], [PSK[bk]])
                yield
                for h in range(4):
                    ts("dve", dmx[:, hs4(h)], PS[bk][:, hs4(h)], gc[:, h:h + 1], ALU.subtract, [PSK[bk], gsk], ["dmx"], s2=0.0, op1=ALU.max)
                act(E_, dmx, AF.Exp, ["dmx"], ["E"], scale=-1.0)
                yield
                tt("pool", EL, E_, M4s, ALU.mult, ["E", "M4s"], ["EL"])
                tt("pool", EQ, E_, M4i, ALU.mult, ["E", "M4i"], ["EQ"])
                bkk = nbA2()
                for h in range(4):
                    mm(PS[bkk][:, hs4(h)], kn_[:, hs4(h)], kn_[:, hs4(h)], True, True, [knk], [PSK[bkk]])
                bqk = nbA2()
                for h in range(4):
                    mm(PS[bqk][:, hs4(h)], qn_[:, hs4(h)], kn_[:, hs4(h)], True, True, [qnk, knk], [PSK[bqk]])
                yield
                tt("dve", L_, PS[bkk], EL, ALU.mult, [PSK[bkk], "EL"], ["L"])
                tt("dve", b3(L_), b3(L_), bc_free(beta, 128), ALU.mult, ["L", gsk], ["L"])
                tt("dve", QK, PS[bqk], EQ, ALU.mult, [PSK[bqk], "EQ"], ["QK"])
                yield
                bt = nbA2()
                for h in range(4):
                    tr(PS[bt][:, hs4(h)], L_[:, hs4(h)], identf, ["L", "identf"], [PSK[bt]])
                cp("act", Mb[0], PS[bt], [PSK[bt]], ["M0"])
                bt = nbA2()
                for h in range(4):
                    tr(PS[bt][:, hs4(h)], QK[:, hs4(h)], identf, ["QK", "identf"], [PSK[bt]])
                cp("act", QKTS[s2], PS[bt], [PSK[bt]], [f"QKT{s2}"])
                yield
                tt("dve", b3(Xb[0]), ident4, b3(Mb[0]), ALU.subtract, ["identf", "M0"], ["X0"])
                xc = 0
                Ncur, Nk_ = L_, "L"
                Mcur, Mk_ = Mb[0], "M0"
                for lv in range(1, 6):
                    Nn, Nnk = Nb[lv % 2], f"N{lv % 2}"
                    bn = nbA2()
                    for h in range(4):
                        mm(PS[bn][:, hs4(h)], Mcur[:, hs4(h)], Ncur[:, hs4(h)], True, True, [Mk_, Nk_], [PSK[bn]])
                    cp("act", Nn, PS[bn], [PSK[bn]], [Nnk])
                    if lv < 5:
                        Mn, Mnk = Mb[lv % 2], f"M{lv % 2}"
                        bm = nbA2()
                        for h in range(4):
                            mm(PS[bm][:, hs4(h)], Ncur[:, hs4(h)], Mcur[:, hs4(h)], True, True, [Mk_, Nk_], [PSK[bm]])
                    yield
                    bx = nbA2()
                    for h in range(4):
                        mm(PS[bx][:, hs4(h)], Nn[:, hs4(h)], Xb[xc][:, hs4(h)], True, True, [Nnk, f"X{xc}"], [PSK[bx]])
                    tt("dve", Xb[1 - xc], Xb[xc], PS[bx], ALU.add, [f"X{xc}", PSK[bx]], [f"X{1 - xc}"])
                    xc = 1 - xc
                    if lv < 5:
                        cp("act", Mn, PS[bm], [PSK[bm]], [Mnk])
                        Mcur, Mk_ = Mn, Mnk
                    Ncur, Nk_ = Nn, Nnk
                    yield
                X_, Xk = Xb[xc], f"X{xc}"
                bkt = nbA2()
                for h in range(4):
                    tr(PS[bkt][:, hs4(h)], kn_[:, hs4(h)], identf, [knk, "identf"], [PSK[bkt]])
                bvt = nbA2()
                for h in range(4):
                    tr(PS[bvt][:, hs4(h)], vv_[:, hs4(h)], identf, [vvk, "identf"], [PSK[bvt]])
                yield
                tt("dve", b3(vb), b3(PS[bvt]), bc_free(beta, 128), ALU.mult, [PSK[bvt], gsk], ["vb"])
                tt("dve", b3(kbg), b3(PS[bkt]), bc_free(bkeg, 128), ALU.mult, [PSK[bkt], gsk], ["kbg"])
                tt("dve", b3(ktS[s2]), b3(PS[bkt]), bc_free(ktw, 128), ALU.mult, [PSK[bkt], gsk], [f"kt{s2}"])
                yield
                bval = nbA2()
                for h in range(4):
                    mm(PS[bval][:, hs4(h)], X_[:, hs4(h)], vb[:, hs4(h)], True, True, [Xk, "vb"], [PSK[bval]])
                bkc = nbA2()
                for h in range(4):
                    mm(PS[bkc][:, hs4(h)], kbg[:, hs4(h)], X_[:, hs4(h)], True, True, [Xk, "kbg"], [PSK[bkc]])
                cp("act", valS[s2], PS[bval], [PSK[bval]], [f"val{s2}"])
                cp("act", kcTS[s2], PS[bkc], [PSK[bkc]], [f"kcT{s2}"])
                yield

            def stageB(t):
                own = t >= 16
                s3 = t % 3
                s2 = t % 2
                gs = gsS[s3]
                eg = gs[:, 20:24]; eglB = gs[:, 28:36]
                gsk = f"gs{s3}"
                qn_ = qnS[s3]; qnk = f"qn{s3}"
                QKT_, kt_, value_, kcT_ = QKTS[s2], ktS[s2], valS[s2], kcTS[s2]
                for blk in range(2):
                    r_ = slice(64 * blk, 64 * blk + 64)
                    c0 = 64 * blk
                    bv = nbB()
                    for h in range(4):
                        mm(PS[bv][r_, hs4(h)], kcT_[:, h * 128 + c0:h * 128 + c0 + 64], S_[:, hs4(h)], True, True, [f"kcT{s2}", "S"], [PSK[bv]], tp=(0, c0))
                    tt("dve", vnew[r_, :], value_[r_, :], PS[bv][r_, :], ALU.subtract, [f"val{s2}", PSK[bv]], ["vnew"])
                    yield
                    if own:
                        bo1 = nbB()
                        for h in range(4):
                            mm(PS[bo1][r_, hs4(h)], qn_[:, h * 128 + c0:h * 128 + c0 + 64], S_[:, hs4(h)], True, True, [qnk, "S"], [PSK[bo1]], tp=(0, c0))
                        bo2 = nbB()
                        for h in range(4):
                            mm(PS[bo2][r_, hs4(h)], QKT_[r_, h * 128 + c0:h * 128 + c0 + 64], vnew[r_, hs4(h)], True, True, [f"QKT{s2}", "vnew"], [PSK[bo2]], tp=(c0, c0))
                    bs = nbB()
                    for h in range(4):
                        mm(PS[bs][:, hs4(h)], kt_[r_, hs4(h)], vnew[r_, hs4(h)], True, True, [f"kt{s2}", "vnew"], [PSK[bs]], tp=(c0, 0))
                    tt("dve", b3(Stmp), b3(S_), bc_free(eglB[:, blk * 4:(blk + 1) * 4], 128), ALU.mult, ["S", gsk], ["Stmp"])
                    tt("dve", S_, Stmp, PS[bs], ALU.add, ["Stmp", PSK[bs]], ["S"])
                    yield
                    if own:
                        tt("dve", b3(otmp[r_, :]), b3(PS[bo1][r_, :]), eg[r_, :].unsqueeze(2).broadcast_to([64, 4, 128]), ALU.mult, [PSK[bo1], gsk], ["otmp"])
                        tt("dve", o_t[r_, :], otmp[r_, :], PS[bo2][r_, :], ALU.add, ["otmp", PSK[bo2]], ["o_t"])
                        yield
                if own:
                    act(otmp, o_t, AF.Square, ["o_t"], ["otmp"])
                    P.op("dve", lambda e: e.reduce_sum(out=oss, in_=b3(otmp), axis=AX.X), r=["otmp"], w=["oss"], cost=0.6)
                    act(orstd, oss, AF.Ln, ["oss", "eps"], ["orstd"], bias=eps_t, scale=1.0 / 128)
                    act(orstd, orstd, AF.Exp, ["orstd"], ["orstd"], scale=-0.5)
                    yield
                    tt("dve", b3(otmp), b3(o_t), bc_free(orstd, 128), ALU.mult, ["o_t", "orstd"], ["otmp"])
                    tt("pool", b3(otmp), b3(otmp), gngB.unsqueeze(1).broadcast_to([128, 4, 128]), ALU.mult, ["otmp", "gngB"], ["otmp"])
                    tt("dve", ygd, otmp, zsS[s3], ALU.mult, ["otmp", f"zs{s3}"], ["ygd"])
                    yield
                    bk = nbB()
                    pb = PS[bk].bitcast(BF16)
                    for h in range(4):
                        tr(pb[:, hs4(h)], ygd[:, hs4(h)], identb, ["ygd", "identb"], [PSK[bk]])
                    cp("act", yT[:, 4:8, (t - 16) * 128:(t - 16 + 1) * 128], b3(pb[:, 0:512]), [PSK[bk]], ["yT"])
                    yield

            NT_ = 32
            for it in range(NT_ + 2):
                active = []
                if it < NT_:
                    active.append(stageA1(it))
                if 0 <= it - 1 < NT_:
                    active.append(stageA2(it - 1))
                if 0 <= it - 2 < NT_:
                    active.append(stageB(it - 2))
                while active:
                    for g in list(active):
                        try:
                            next(g)
                        except StopIteration:
                            active.remove(g)
            P.barrier("G")
            A.off = mGd


        if "C" in phases:
            A.off = after_yT
            acc = A.f32(16 * D).rearrange("p (t n) -> p t n", t=16)
            hT2 = A.bf16(8 * NOWN).rearrange("p (k n) -> p k n", k=8)
            comb = A.f32(16 * 32).rearrange("p (t e) -> p t e", t=16)
            gt2B = A.f32(D); gfB = A.f32(D)
            mC1 = A.off
            gt1B = A.f32(D); geff2 = A.f32(D); sh2B = A.f32(D); g2B = A.f32(D)
            diag_slots = [A.f32(128), A.f32(128)]
            wout = A.bf16(8 * D).rearrange("p (k n) -> p k n", k=8)
            wr = A.f32(8 * 36).rearrange("p (k n) -> p k n", k=8)
            xt2 = [A.f32(D), A.f32(D)]
            tmpc = A.f32(D); mf = A.f32(D)
            hTf = A.f32(8 * 128).rearrange("p (k n) -> p k n", k=8)
            ss2 = A.f32(1); rstd2 = A.f32(1)
            lg = A.f32(36); gmax = A.f32(1); oh = A.f32(4); negm = A.f32(1); ex4 = A.f32(4); sum4 = A.f32(1); pg = A.f32(1)
            selv = A.f32(8); sel2 = A.f32(8); m1 = A.f32(1); m2 = A.f32(1); oh1 = A.f32(8); oh2 = A.f32(8)
            dm = A.f32(1); w1 = A.f32(1); w2 = A.f32(1); c8 = A.f32(8)

            dma("sp", g2B, norm2_g.partition_broadcast(128), w=["g2B"])
            dma("sp", gfB, normf_g.partition_broadcast(128), w=["gfB"])
            for k in range(8):
                dma("pool", wout[:, k, :], w_out[k * 128:(k + 1) * 128, :], w=["wout"], dsem="wout")
                dma("sp", wr[:, k, 0:4], w_rg[k * 128:(k + 1) * 128, :], w=["wr"])
                dma("sp", wr[:, k, 4:36], w_re[k * 128:(k + 1) * 128, :], w=["wr"])
            bcast_row(lambda h, ps, pk: cp("act", gt1B[:, h * 512:(h + 1) * 512], ps, [pk], ["gt1B"]), modT[:, 16:24], "modT")
            bcast_row(lambda h, ps, pk: cp("act", sh2B[:, h * 512:(h + 1) * 512], ps, [pk], ["sh2B"]), modT[:, 24:32], "modT")
            bcast_row(lambda h, ps, pk: stt("dve", geff2[:, h * 512:(h + 1) * 512], ps, 1.0, g2B[:, h * 512:(h + 1) * 512],
                                            ALU.add, ALU.mult, [pk, "g2B"], ["geff2"]), modT[:, 32:40], "modT")
            bcast_row(lambda h, ps, pk: cp("act", gt2B[:, h * 512:(h + 1) * 512], ps, [pk], ["gt2B"]), modT[:, 40:48], "modT")


            cstop = float(os.environ.get("CSTOP", "9"))
            for t in range(16 if cstop >= 2 else 0):
                sl = t % 2
                dma("sp", xt2[sl], xs[(16 + t) * 128:(17 + t) * 128, :], w=[f"xt2{sl}"], dsem=f"xt2{sl}")
                for nh in range(2):
                    bk = nb()
                    nsl = slice(nh * 512, (nh + 1) * 512)
                    for k in range(8):
                        mm(PS[bk], yT[:, k, t * 128:(t + 1) * 128], wout[:, k, nsl], k == 0, k == 7, ["yT", "wout"], [PSK[bk]])
                    tt("dve", tmpc[:, nsl], PS[bk], gt1B[:, nsl], ALU.mult, [PSK[bk], "gt1B"], ["tmpc"])
                x1 = acc[:, t, :]
                ak = [f"acc{t}0", f"acc{t}1"]
                tt("dve", x1, tmpc, xt2[sl], ALU.add, ["tmpc", f"xt2{sl}"], ak)
                if cstop < 3:
                    continue
                act(tmpc, x1, AF.Square, ak, ["tmpc", "ss2"], accum=ss2)
                act(rstd2, ss2, AF.Ln, ["ss2", "eps"], ["rstd2"], bias=eps_t, scale=1.0 / D)
                act(rstd2, rstd2, AF.Exp, ["rstd2"], ["rstd2"], scale=-0.5)
                stt("dve", mf, x1, rstd2, geff2, ALU.mult, ALU.mult, ak + ["rstd2", "geff2"], ["mf"])
                tt("dve", mf, mf, sh2B, ALU.add, ["mf", "sh2B"], ["mf"])
                if cstop < 3.2:
                    continue
                for half in range(2):
                    bk = nb()
                    for k4 in range(4):
                        k = half * 4 + k4
                        tr(PS[bk][:, k4 * 128:(k4 + 1) * 128], mf[:, k * 128:(k + 1) * 128], identf, ["mf", "identf"], [PSK[bk]])
                    if cstop >= 3.4:
                        cp("act", hTf[:, half * 4:(half + 1) * 4, :], b3(PS[bk]), [PSK[bk]], ["hTf"])
                    if cstop >= 3.6:
                        cp("pool", hT2[:, half * 4:(half + 1) * 4, t * 128:(t + 1) * 128], hTf[:, half * 4:(half + 1) * 4, :], ["hTf"], ["hT2"])
                if cstop < 4:
                    continue
                bk = nb()
                for k in range(8):
                    mm(PS[bk][:, 0:36], hTf[:, k, :], wr[:, k, :], k == 0, k == 7, ["hTf", "wr"], [PSK[bk]])
                cp("dve", lg, PS[bk][:, 0:36], [PSK[bk]], ["lg"])
                P.op("dve", lambda e: e.reduce_max(out=gmax, in_=lg[:, 0:4], axis=AX.X), r=["lg"], w=["gmax"])
                ts("dve", oh, lg[:, 0:4], gmax, ALU.is_equal, ["lg", "gmax"], ["oh"])
                ts("dve", negm, gmax, -1.0, ALU.mult, ["gmax"], ["negm"])
                act(ex4, lg[:, 0:4], AF.Exp, ["lg", "negm"], ["ex4", "sum4"], bias=negm, accum=sum4)
                P.op("dve", lambda e: e.reciprocal(out=pg, in_=sum4), r=["sum4"], w=["pg"])
                ts("dve", selv, lg[:, 4:12], oh[:, 0:1], ALU.mult, ["lg", "oh"], ["selv"])
                for g in range(1, 4):
                    stt("dve", selv, lg[:, 4 + 8 * g:12 + 8 * g], oh[:, g:g + 1], selv, ALU.mult, ALU.add, ["lg", "oh", "selv"], ["selv"])
                P.op("dve", lambda e: e.reduce_max(out=m1, in_=selv, axis=AX.X), r=["selv"], w=["m1"])
                ts("dve", oh1, selv, m1, ALU.is_equal, ["selv", "m1"], ["oh1"])
                stt("dve", sel2, oh1, -1.0e30, selv, ALU.mult, ALU.add, ["oh1", "selv"], ["sel2"])
                P.op("dve", lambda e: e.reduce_max(out=m2, in_=sel2, axis=AX.X), r=["sel2"], w=["m2"])
                ts("dve", oh2, sel2, m2, ALU.is_equal, ["sel2", "m2"], ["oh2"])
                tt("dve", dm, m1, m2, ALU.subtract, ["m1", "m2"], ["dm"])
                act(w1, dm, AF.Exp, ["dm"], ["w1"], scale=-1.0)
                ts("dve", w1, w1, 1.0, ALU.add, ["w1"], ["w1"])
                P.op("dve", lambda e: e.reciprocal(out=w1, in_=w1), r=["w1"], w=["w1"])
                ts("dve", w2, w1, -1.0, ALU.mult, ["w1"], ["w2"], s2=1.0, op1=ALU.add)
                tt("dve", w1, w1, pg, ALU.mult, ["w1", "pg"], ["w1"])
                tt("dve", w2, w2, pg, ALU.mult, ["w2", "pg"], ["w2"])
                ts("dve", c8, oh1, w1, ALU.mult, ["oh1", "w1"], ["c8"])
                stt("dve", c8, oh2, w2, c8, ALU.mult, ALU.add, ["oh2", "w2", "c8"], ["c8"])
                for g in range(4):
                    ts("dve", comb[:, t, g * 8:(g + 1) * 8], c8, oh[:, g:g + 1], ALU.mult, ["c8", "oh"], ["comb"])
            if "x1" in dbg:
                o = nc.dram_tensor("dbg_x1", [128, 16 * D], F32, kind="ExternalOutput").ap()
                dbg_out["x1"] = o
                dma("sp", o, acc.rearrange("p t n -> p (t n)"), r=[f"acc{t}{h}" for t in range(16) for h in range(2)], dsem="dbgst")
                o = nc.dram_tensor("dbg_comb", [128, 16 * 32], F32, kind="ExternalOutput").ap()
                dbg_out["comb"] = o
                dma("sp", o, comb.rearrange("p t n -> p (t n)"), r=["comb"], dsem="dbgst")
            A1 = Arena(arena_t[:, yT_mark:after_yT], after_yT - yT_mark)
            wg = [A1.bf16(8 * 256).rearrange("p (k n) -> p k n", k=8) for _ in range(2)]
            wu = [A1.bf16(8 * 256).rearrange("p (k n) -> p k n", k=8) for _ in range(2)]
            wd = [A1.bf16(2 * D).rearrange("p (f n) -> p f n", f=2) for _ in range(2)]
            n_exp = int(os.environ.get('NEXP', '32'))

            def load_expert(e_, extra_r=()):
                sl = e_ % 2
                dma("pool", wg[sl], w_gate[e_].rearrange("(k p) n -> p k n", p=128), r=list(extra_r), w=[f"wg{sl}"], dsem=f"wg{sl}")
                dma("pool", wu[sl], w_up[e_].rearrange("(k p) n -> p k n", p=128), r=list(extra_r), w=[f"wu{sl}"], dsem=f"wu{sl}")
                dma("pool", wd[sl], w_down[e_].rearrange("(f p) n -> p f n", p=128), r=list(extra_r), w=[f"wd{sl}"], dsem=f"wd{sl}")

            guard = A.f32(1)
            memset("pool", guard, 0.0, ["yT", "yTguard"])
            for e_ in range(min(2, n_exp)):
                load_expert(e_, ["yTguard"])
            P.barrier("C1")
            A.off = mC1
            actT = [A.bf16(2 * NOWN).rearrange("p (f n) -> p f n", f=2) for _ in range(2)]
            sg = [A.f32(512), A.f32(512)]
            ot = [A.f32(D), A.f32(D)]
            sqj = A.f32(D); ss3 = A.f32(1); rstd3 = A.f32(1)

            cnt = 0
            for e_ in range(n_exp):
                sl = e_ % 2
                if e_ >= 2:
                    load_expert(e_)
                tt("pool", wd[sl], wd[sl], gt2B.unsqueeze(1).broadcast_to([128, 2, D]), ALU.mult, [f"wd{sl}", "gt2B"], [f"wd{sl}"])
                for ct in range(4):
                    csl = slice(ct * 512, (ct + 1) * 512)
                    for ft in range(2):
                        fsl = slice(ft * 128, (ft + 1) * 128)
                        bg = nb()
                        for k in range(8):
                            mm(PS[bg], wg[sl][:, k, fsl], hT2[:, k, csl], k == 0, k == 7, [f"wg{sl}", "hT2"], [PSK[bg]])
                        bu = nb()
                        for k in range(8):
                            mm(PS[bu], wu[sl][:, k, fsl], hT2[:, k, csl], k == 0, k == 7, [f"wu{sl}", "hT2"], [PSK[bu]])
                        s2_ = cnt % 2
                        cnt += 1
                        act(sg[s2_], PS[bg], AF.Silu, [PSK[bg]], [f"sg{s2_}"])
                        tt("dve", actT[sl][:, ft, csl], sg[s2_], PS[bu], ALU.mult, [f"sg{s2_}", PSK[bu]], [f"actT{sl}{ct}"])
                for t in range(16):
                    for nh in range(2):
                        nsl = slice(nh * 512, (nh + 1) * 512)
                        bd = nb()
                        for ft in range(2):
                            mm(PS[bd], actT[sl][:, ft, t * 128:(t + 1) * 128], wd[sl][:, ft, nsl], ft == 0, ft == 1,
                               [f"actT{sl}{t // 4}", f"wd{sl}"], [PSK[bd]])
                        stt("dve", acc[:, t, nsl], PS[bd], comb[:, t, e_:e_ + 1], acc[:, t, nsl], ALU.mult, ALU.add,
                            [PSK[bd], "comb", f"acc{t}{nh}"], [f"acc{t}{nh}"])
            for t in range(16):
                sl = t % 2
                ak = [f"acc{t}0", f"acc{t}1"]
                act(sqj, acc[:, t, :], AF.Square, ak, ["sqj", "ss3"], accum=ss3)
                act(rstd3, ss3, AF.Ln, ["ss3", "eps"], ["rstd3"], bias=eps_t, scale=1.0 / D)
                act(rstd3, rstd3, AF.Exp, ["rstd3"], ["rstd3"], scale=-0.5)
                stt("dve", ot[sl], acc[:, t, :], rstd3, gfB, ALU.mult, ALU.mult, ak + ["rstd3", "gfB"], [f"ot{sl}"])
                dma("sp", out_d[t * 128:(t + 1) * 128, :], ot[sl], r=[f"ot{sl}"], dsem="out")

        if "yT" in dbg:
            o = nc.dram_tensor("dbg_yT", [128, 8 * NOWN], F32, kind="ExternalOutput").ap()
            dbg_out["yT"] = o
            ytmp = A.f32(8 * 512)
            yTflat = yT.rearrange("p k n -> p (k n)")
            for c in range(4):
                cp("dve", ytmp, yTflat[:, c * 4096:(c + 1) * 4096], ["yT"], ["ytmp"])
                dma("sp", o[:, c * 4096:(c + 1) * 4096], ytmp, r=["ytmp"], dsem="dbgst")

        for name in list(P.dcount):
            pass
        P.barrier("C2")
        P.emit(st)
    return nc, dbg_out


def _prep_inputs(inputs):
    x = np.ascontiguousarray(inputs["x"], dtype=np.float32)
    shared = {
        "norm1_g": inputs["norm1_g"].reshape(1, D), "norm2_g": inputs["norm2_g"].reshape(1, D),
        "normf_g": inputs["normf_g"].reshape(1, D),
        "w_ada": inputs["w_ada"][0], "b_ada": inputs["b_ada"][0], "w_in": inputs["w_in"][0],
        "lam_re": inputs["lam_re"][0], "lam_im": inputs["lam_im"][0], "log_step": inputs["log_step"][0],
        "s5_b_re": inputs["s5_b_re"][0], "s5_b_im": inputs["s5_b_im"][0],
        "s5_c_re": inputs["s5_c_re"][0], "s5_c_im": inputs["s5_c_im"][0],
        "s5_d": inputs["s5_d"][0], "w_glu": inputs["w_glu"][0], "b_glu": inputs["b_glu"][0],
        "conv_w": inputs["conv_w"][0], "a_log": inputs["a_log"].reshape(1, 4), "dt_bias": inputs["dt_bias"].reshape(1, 4),
        "gdn_norm_g": inputs["gdn_norm_g"].reshape(1, 128), "w_out": inputs["w_out"][0],
        "w_router_grp": inputs["w_router_grp"][0], "w_router_exp": inputs["w_router_exp"][0],
        "w_gate": inputs["w_gate"][0], "w_up": inputs["w_up"][0], "w_down": inputs["w_down"][0],
    }
    shared = {k: np.ascontiguousarray(v, dtype=np.float32) for k, v in shared.items()}
    in_maps = []
    for core in range(8):
        b, half = core // 2, core % 2
        if half == 0:
            xs_ = np.concatenate([np.zeros((NOWN, D), np.float32), x[b, :NOWN]], axis=0)
        else:
            xs_ = x[b]
        m = dict(shared)
        m["xs"] = np.ascontiguousarray(xs_)
        m["flag"] = np.full((128, 1), float(half), np.float32)
        m["cb"] = np.ascontiguousarray(inputs["c"][b:b + 1], dtype=np.float32)
        in_maps.append(m)
    return in_maps


def kernel(**inputs):
    nc, _ = build()
    in_maps = _prep_inputs(inputs)
    res = run_bass_kernel_spmd(nc, in_maps, core_ids=list(range(8)))
    out = np.zeros((4, 4096, D), np.float32)
    for core in range(8):
        b, half = core // 2, core % 2
        out[b, half * NOWN:(half + 1) * NOWN] = res.results[core]["out"]
    return out
```

```python
import math
import os
from contextlib import ExitStack

import numpy as np
import concourse.bass as bass
import concourse.mybir as mybir
from concourse.bass_utils import run_bass_kernel_spmd

F32 = mybir.dt.float32
BF16 = mybir.dt.bfloat16
I32 = mybir.dt.int32
AF = mybir.ActivationFunctionType
ALU = mybir.AluOpType
AX = mybir.AxisListType

ENGS = ["sp", "act", "dve", "pool", "pe"]
SEM_SPAN = 30000


class _Op:
    __slots__ = ("eng", "fn", "deps", "dsem", "dcount", "need_sig", "sig", "dma_waits", "alld", "cost", "lat",
                 "idx", "succ", "npred", "prio", "rt", "fin", "grp")

    def __init__(self, eng, fn, dsem):
        self.eng = eng
        self.fn = fn
        self.deps = []
        self.dma_waits = []
        self.alld = []
        self.dsem = dsem
        self.dcount = 0
        self.need_sig = False
        self.sig = None
        self.cost = 0.1
        self.lat = 0.0
        self.grp = None


HOP = 0.7
SEG_HOP = {"G": 0.85, "S_setup": 1.1, "S_p1": 1.1, "S_p23": 1.1}
ACT_SWITCH = 1.3


class Prog:
    def __init__(self, nc):
        self.nc = nc
        self.cur = {e: [] for e in ENGS}
        self.segs = []
        self.last_w = {}
        self.readers = {}
        self.dcount = {}
        self.last_dma = {}
        self.nops = 0
        self.sched = True

    def _dep(self, op, tok):
        if tok is not None and tok is not op:
            op.alld.append(tok)

    def op(self, eng, fn, r=(), w=(), dsem=None, cost=0.1, lat=0.0, grp=None):
        o = _Op(eng, fn, dsem)
        o.cost = cost
        o.lat = lat
        o.grp = grp
        o.idx = self.nops
        self.nops += 1
        for k in r:
            self._dep(o, self.last_w.get(k))
        for k in w:
            self._dep(o, self.last_w.get(k))
            for t in self.readers.get(k, ()):
                self._dep(o, t)
        if dsem is not None:
            self.dcount[dsem] = self.dcount.get(dsem, 0) + 16
            o.dcount = self.dcount[dsem]
            prev = self.last_dma.get(dsem)
            if prev is not None:
                assert prev.eng == eng, f"dsem {dsem} used from two queues"
                self._dep(o, prev)
            self.last_dma[dsem] = o
        for k in r:
            self.readers.setdefault(k, []).append(o)
        for k in w:
            self.last_w[k] = o
            self.readers[k] = []
        self.cur[eng].append(o)
        return o

    def barrier(self, name=None):
        self.segs.append((self.cur, dict(self.dcount), name))
        self.cur = {e: [] for e in ENGS}
        self.last_w.clear()
        self.readers.clear()
        self.last_dma.clear()

    def _schedule(self, seg, HOP=HOP):
        import heapq
        ops = [o for e in ENGS for o in seg[e]]
        if not ops:
            return seg
        ops.sort(key=lambda o: o.idx)
        inseg = set(id(o) for o in ops)
        for o in ops:
            o.succ = []
        for o in ops:
            seen = set()
            preds = []
            for d in o.alld:
                if id(d) in inseg and id(d) not in seen:
                    seen.add(id(d))
                    preds.append(d)
            o.alld = preds
            o.npred = len(preds)
            for d in preds:
                d.succ.append(o)
        for o in reversed(ops):
            m = 0.0
            for s in o.succ:
                h = 0.0 if (o.eng == "pe" and s.eng == "pe") else HOP
                if s.prio + h > m:
                    m = s.prio + h
            o.prio = o.cost + o.lat + m
            o.rt = 0.0
        free = {e: 0.0 for e in ENGS}
        pend = {e: [] for e in ENGS}
        ready = {e: [] for e in ENGS}
        order = {e: [] for e in ENGS}
        act_cur = [None]
        for o in ops:
            if o.npred == 0:
                heapq.heappush(pend[o.eng], (0.0, -o.prio, o.idx, o))
        n = len(ops)
        while n:
            best = None
            for e in ENGS:
                pe_, re_ = pend[e], ready[e]
                f = free[e]
                while pe_ and pe_[0][0] <= f:
                    it = heapq.heappop(pe_)
                    heapq.heappush(re_, (it[1], it[2], it[3]))
                if re_:
                    c = f
                elif pe_:
                    c = pe_[0][0]
                else:
                    continue
                if best is None or c < best[0]:
                    best = (c, e)
            c, e = best
            if ready[e]:
                if e == "act" and len(ready[e]) > 1:
                    top = heapq.nsmallest(6, ready[e])
                    pick = top[0]
                    if pick[2].grp is not None and pick[2].grp != act_cur[0]:
                        for it in top[1:]:
                            if (it[2].grp is None or it[2].grp == act_cur[0]) and it[0] <= pick[0] + 4.0:
                                pick = it
                                break
                    ready[e].remove(pick)
                    heapq.heapify(ready[e])
                    o = pick[2]
                else:
                    o = heapq.heappop(ready[e])[2]
            else:
                o = heapq.heappop(pend[e])[3]
            st_ = max(free[e], o.rt)
            cost = o.cost
            if e == "act" and o.grp is not None and o.grp != act_cur[0]:
                cost += ACT_SWITCH
                act_cur[0] = o.grp
            o.fin = st_ + cost
            free[e] = o.fin
            order[e].append(o)
            n -= 1
            for s in o.succ:
                h = 0.0 if (e == "pe" and s.eng == "pe") else HOP
                t_ = o.fin + o.lat + h
                if t_ > s.rt:
                    s.rt = t_
                s.npred -= 1
                if s.npred == 0:
                    heapq.heappush(pend[s.eng], (s.rt, -s.prio, s.idx, s))
        self.est_us = getattr(self, "est_us", 0.0) + max(free.values())
        return order

    def emit(self, stack):
        nc = self.nc
        if any(self.cur[e] for e in ENGS):
            self.barrier()
        final = {e: [] for e in ENGS}
        for seg, dc, name in self.segs:
            order = self._schedule(seg, SEG_HOP.get(name, HOP)) if self.sched else seg
            for e in ENGS:
                for o in order[e]:
                    for d in o.alld:
                        if d.dsem is not None:
                            if d.eng == o.eng and o.dsem == d.dsem:
                                continue
                            o.dma_waits.append((d.dsem, d.dcount))
                        elif d.eng == "pe" and o.eng == "pe":
                            continue
                        else:
                            d.need_sig = True
                            o.deps.append(d)
                    final[e].append(o)
            lasts = {}
            for e in ENGS:
                for o in reversed(final[e]):
                    if o.fn is not None and o.dsem is None:
                        lasts[e] = o
                        break
            for e in ENGS:
                f = _Op(e, None, None)
                for e2, o in lasts.items():
                    if e2 != e:
                        o.need_sig = True
                        f.deps.append(o)
                for name, cnt in dc.items():
                    f.dma_waits.append((name, cnt))
                final[e].append(f)
        esems = {}
        for e in ENGS:
            n = 0
            for o in final[e]:
                if o.need_sig and o.fn is not None and o.dsem is None:
                    o.sig = n
                    n += 1
            nsem = max(1, (n + SEM_SPAN - 1) // SEM_SPAN)
            esems[e] = [stack.enter_context(nc.semaphore(f"s_{e}{i}")) for i in range(nsem)]
        dsems = {name: stack.enter_context(nc.semaphore(f"d_{name}")) for name in self.dcount}
        block = stack.enter_context(nc.Block())

        def run(ename, eng):
            waited = {}
            for o in final[ename]:
                for d in o.deps:
                    if d.sig is None:
                        continue
                    key = (d.eng, d.sig // SEM_SPAN)
                    val = d.sig % SEM_SPAN + 1
                    if waited.get(key, 0) >= val:
                        continue
                    waited[key] = val
                    eng.wait_ge(esems[d.eng][key[1]], val)
                for (name, val) in o.dma_waits:
                    key = ("dma", name)
                    if waited.get(key, 0) >= val:
                        continue
                    waited[key] = val
                    eng.wait_ge(dsems[name], val)
                if o.fn is None:
                    continue
                ins = o.fn(eng)
                if o.dsem is not None:
                    ins.then_inc(dsems[o.dsem], 16)
                elif o.sig is not None:
                    ins.then_inc(esems[ename][o.sig // SEM_SPAN], 1)

        @block.sync
        def _(e):
            run("sp", e)

        @block.scalar
        def _(e):
            run("act", e)

        @block.vector
        def _(e):
            run("dve", e)

        @block.gpsimd
        def _(e):
            run("pool", e)

        @block.tensor
        def _(e):
            run("pe", e)


class Arena:
    def __init__(self, ap, n):
        self.ap = ap
        self.n = n
        self.off = 0

    def f32(self, n):
        a = self.ap[:, self.off:self.off + n]
        self.off += n
        assert self.off <= self.n, f"arena overflow {self.off} > {self.n}"
        return a

    def bf16(self, n):
        m = (n + 1) // 2
        return self.f32(m).bitcast(BF16)

    def i32(self, n):
        return self.f32(n).bitcast(I32)


D = 1024
NTOK = 4096
NOWN = 2048
EPS = 1e-6
TWO_PI = 2.0 * math.pi
PI_LO = 3.14159
ARENA_N = 53200


def dram_ap(t, offset, dims):
    return bass.AP(t.tensor, t.offset + offset, [list(d) for d in dims])


def build(phases=("0", "S", "G", "C"), dbg=()):
    nc = bass.Bass("TRN2", target_bir_lowering=False)

    def din(name, shape):
        return nc.dram_tensor(name, list(shape), F32, kind="ExternalInput").ap()

    xs = din("xs", [NTOK, D])
    flag_d = din("flag", [128, 1])
    cb = din("cb", [1, D])
    norm1_g = din("norm1_g", [1, D])
    norm2_g = din("norm2_g", [1, D])
    normf_g = din("normf_g", [1, D])
    w_ada = din("w_ada", [D, 6 * D])
    b_ada = din("b_ada", [6 * D])
    w_in = din("w_in", [D, 2568])
    lam_re = din("lam_re", [32, 64])
    lam_im = din("lam_im", [32, 64])
    log_step = din("log_step", [32])
    s5_b_re = din("s5_b_re", [32, 64, 16])
    s5_b_im = din("s5_b_im", [32, 64, 16])
    s5_c_re = din("s5_c_re", [32, 16, 64])
    s5_c_im = din("s5_c_im", [32, 16, 64])
    s5_d = din("s5_d", [512])
    w_glu = din("w_glu", [512, 512])
    b_glu = din("b_glu", [512])
    conv_w = din("conv_w", [4, 1536])
    a_log = din("a_log", [1, 4])
    dt_bias = din("dt_bias", [1, 4])
    gdn_norm_g = din("gdn_norm_g", [1, 128])
    w_out = din("w_out", [D, D])
    w_rg = din("w_router_grp", [D, 4])
    w_re = din("w_router_exp", [D, 32])
    w_gate = din("w_gate", [32, D, 256])
    w_up = din("w_up", [32, D, 256])
    w_down = din("w_down", [32, 256, D])
    out_d = nc.dram_tensor("out", [NOWN, D], F32, kind="ExternalOutput").ap()
    dbg_out = {}

    st = ExitStack()
    with st:
        arena_t = st.enter_context(nc.sbuf_tensor("arena", [128, ARENA_N], F32))
        A = Arena(arena_t[:], ARENA_N)
        PS = [st.enter_context(nc.psum_tensor(f"psb{i}", [128, 512], F32))[:] for i in range(8)]
        PSK = [f"ps{i}" for i in range(8)]
        P = Prog(nc)
        uid = [0]

        def fsz(ap):
            n = 1
            for s in ap.shape[1:]:
                n *= s
            return n

        ACT_GRP = {AF.Exp: "E", AF.Ln: "E", AF.Sigmoid: "G", AF.Silu: "S", AF.Sqrt: "Q", AF.Sin: "N", AF.Gelu_apprx_tanh: "U"}

        def c_dve(out):
            return 0.07 + 0.00105 * fsz(out)

        def c_pool(out):
            return 0.12 + 0.0022 * fsz(out)

        def c_act(out):
            return 0.2 + 0.00075 * fsz(out)

        def c_eng(eng, out):
            return {"dve": c_dve, "pool": c_pool, "act": c_act}[eng](out)

        def c_pe(lhsT, rhs):
            n = fsz(rhs)
            per = 0.00045 if rhs.dtype == BF16 else 0.00118
            return max(0.11, 0.03 + per * n)

        def dma(eng, out, in_, r=(), w=(), dsem=None, slow=False):
            if dsem is None:
                dsem = "k_" + (w[0] if w else r[0])
            nb_ = 4
            for s in in_.shape:
                nb_ *= s
            lat = 2.0 + nb_ / 180e3
            if slow:
                return P.op("pool", lambda e: e.dma_start(out=out, in_=in_, allow_slow_non_contiguous=True), r=r, w=w, dsem=dsem,
                            cost=0.8, lat=lat + 4.0)
            return P.op(eng, lambda e: e.dma_start(out=out, in_=in_), r=r, w=w, dsem=dsem, cost=0.8 if eng == "pool" else 0.35, lat=lat)

        def mm(out, lhsT, rhs, start, stop, r, w, tp=None):
            if tp is None:
                return P.op("pe", lambda e: e.matmul(out, lhsT=lhsT, rhs=rhs, start=start, stop=stop), r=r, w=w, cost=c_pe(lhsT, rhs))
            return P.op("pe", lambda e: e.matmul(out, lhsT=lhsT, rhs=rhs, start=start, stop=stop, tile_position=tp), r=r, w=w,
                        cost=c_pe(lhsT, rhs))

        def tr(out, in_, ident, r, w):
            return P.op("pe", lambda e: e.transpose(out=out, in_=in_, identity=ident), r=r, w=w, cost=0.2)

        def act(out, in_, func, r, w, bias=None, scale=None, accum=None):
            kw = {}
            if bias is not None:
                kw["bias"] = bias
            if scale is not None:
                kw["scale"] = scale
            if accum is not None:
                kw["accum_out"] = accum
            return P.op("act", lambda e: e.activation(out=out, in_=in_, func=func, **kw), r=r, w=w, cost=c_act(out), grp=ACT_GRP.get(func))

        def tt(eng, out, in0, in1, op, r, w):
            return P.op(eng, lambda e: e.tensor_tensor(out=out, in0=in0, in1=in1, op=op), r=r, w=w, cost=c_eng(eng, out))

        def ts(eng, out, in0, s1, op0, r, w, s2=None, op1=None):
            if op1 is None:
                return P.op(eng, lambda e: e.tensor_scalar(out=out, in0=in0, scalar1=s1, scalar2=None, op0=op0), r=r, w=w, cost=c_eng(eng, out))
            return P.op(eng, lambda e: e.tensor_scalar(out=out, in0=in0, scalar1=s1, scalar2=s2, op0=op0, op1=op1), r=r, w=w,
                        cost=c_eng(eng, out))

        def stt(eng, out, in0, scalar, in1, op0, op1, r, w):
            return P.op(eng, lambda e: e.scalar_tensor_tensor(out=out, in0=in0, scalar=scalar, in1=in1, op0=op0, op1=op1), r=r, w=w,
                        cost=c_eng(eng, out))

        def cp(eng, out, in_, r, w):
            if eng == "act":
                return P.op("act", lambda e: e.copy(out=out, in_=in_), r=r, w=w, cost=c_act(out))
            return P.op(eng, lambda e: e.tensor_copy(out=out, in_=in_), r=r, w=w, cost=c_eng(eng, out))

        def memset(eng, ap, val, w):
            return P.op(eng, lambda e: e.memset(ap, val), w=w, cost=c_eng(eng, ap))

        def dump(name, ap, key, shape):
            if name not in dbg:
                return
            o = nc.dram_tensor("dbg_" + name, list(shape), F32, kind="ExternalOutput").ap()
            dbg_out[name] = o
            dma("sp", o, ap, r=[key], dsem="dbgst")

        bank_ctr = [0]

        def nb():
            b_ = bank_ctr[0] % 8
            bank_ctr[0] += 1
            return b_

        def b3(ap, h=4):
            return ap.rearrange("p (h n) -> p h n", h=h)

        def bc_free(ap, n):
            return ap.unsqueeze(2).broadcast_to([128, ap.shape[1], n])

        stg_ctr = [0]

        def load_T(dst, src, n, key, evac="act"):
            i_ = stg_ctr[0] % 2
            stg_ctr[0] += 1
            stg = stg_slots[i_]
            dma("sp", stg[0:n, :], src, w=[f"stg{i_}"], dsem=f"stg{i_}")
            bk = nb()
            mm(PS[bk][:, 0:n], stg[0:n, :], identf[0:n, 0:n], True, True, [f"stg{i_}", "identf"], [PSK[bk]])
            cp(evac, dst, PS[bk][:, 0:n], [PSK[bk]], [key])

        identf = A.f32(128)
        identb = A.bf16(128)
        ones = A.f32(128)
        eps_t = A.f32(1)
        flag = A.f32(1)
        one_t = A.f32(1)
        memset("pool", identf, 1.0, ["identf"])
        P.op("pool", lambda e: e.affine_select(out=identf, in_=identf, pattern=[[-1, 128]], compare_op=ALU.is_equal,
                                               fill=0.0, base=0, channel_multiplier=1), r=["identf"], w=["identf"])
        cp("dve", identb, identf, ["identf"], ["identb"])
        memset("dve", ones, 1.0, ["ones"])
        memset("dve", eps_t, EPS, ["eps"])
        memset("dve", one_t, 1.0, ["one"])
        dma("sp", flag, flag_d, w=["flag"])
        stg_slots = [A.f32(128), A.f32(128)]
        modT = A.f32(48)
        geff1 = A.f32(D)
        sh1B = A.f32(D)
        consts_mark = A.off

        def sin_of(eng, out, th, tmpf, tmpi, shift, r, w):
            ts(eng, tmpf, th, shift, ALU.add, r, ["_sin_tf"], s2=1.0 / TWO_PI, op1=ALU.mult)
            cp(eng, tmpi, tmpf, ["_sin_tf"], ["_sin_ti"])
            cp(eng, tmpf, tmpi, ["_sin_ti"], ["_sin_tf"])
            ts(eng, tmpf, tmpf, -TWO_PI, ALU.mult, ["_sin_tf"], ["_sin_tf"])
            stt(eng, tmpf, th, shift, tmpf, ALU.add, ALU.add, r + ["_sin_tf"], ["_sin_tf"])
            ts(eng, tmpf, tmpf, -PI_LO, ALU.max, ["_sin_tf"], ["_sin_tf"], s2=PI_LO, op1=ALU.min)
            act(out, tmpf, AF.Sin, ["_sin_tf"], w)

        def bcast_row(dst_fn, col_src, key_src, nt=8):
            for half in range(nt // 4):
                bank = half % 2
                for t4 in range(4):
                    t = half * 4 + t4
                    dg = diag_slots[t % 2]
                    ts("dve", dg, identf, col_src[:, t:t + 1], ALU.mult, ["identf", key_src], [f"diag{t % 2}"])
                    mm(PS[bank][:, t4 * 128:(t4 + 1) * 128], ones, dg, True, True, ["ones", f"diag{t % 2}"], [PSK[bank]])
                dst_fn(half, PS[bank], PSK[bank])

        yT_mark = A.off
        yT = A.bf16(8 * NOWN).rearrange("p (k n) -> p k n", k=8)
        after_yT = A.off

        if "0" in phases:
            A0 = Arena(arena_t[:, yT_mark:after_yT], after_yT - yT_mark)
            cT = A0.f32(8)
            scb = A0.bf16(8)
            badaT = A0.f32(48)
            g1B = A0.f32(D)
            diag_slots = [A0.f32(128), A0.f32(128)]
            wada = [A0.bf16(6144), A0.bf16(6144)]
            load_T(cT, cb.rearrange("o (k p) -> (o k) p", p=128), 8, "cT")
            load_T(badaT, b_ada.rearrange("(m p) -> m p", p=128), 48, "badaT")
            dma("sp", g1B, norm1_g.partition_broadcast(128), w=["g1B"])
            act(scb, cT, AF.Silu, ["cT"], ["scb"])
            for cbk in range(8):
                sl = cbk % 2
                wv = wada[sl].rearrange("p (k n) -> p k n", k=8)
                for k in range(8):
                    dma("pool", wv[:, k, :], w_ada[k * 128:(k + 1) * 128, cbk * 768:(cbk + 1) * 768], w=[f"wada{sl}"], dsem=f"wada{sl}")
                for m6 in range(6):
                    m = cbk * 6 + m6
                    for k in range(8):
                        mm(PS[7][:, m:m + 1], wv[:, k, m6 * 128:(m6 + 1) * 128], scb[:, k:k + 1], k == 0, k == 7,
                           [f"wada{sl}", "scb"], [PSK[7]])
            tt("dve", modT, PS[7][:, 0:48], badaT, ALU.add, [PSK[7], "badaT"], ["modT"])
            dump("modT", modT, "modT", [128, 48])
            bcast_row(lambda h, ps, pk: stt("dve", geff1[:, h * 512:(h + 1) * 512], ps, 1.0, g1B[:, h * 512:(h + 1) * 512],
                                            ALU.add, ALU.mult, [pk, "g1B"], ["geff1"]), modT[:, 8:16], "modT")
            bcast_row(lambda h, ps, pk: cp("act", sh1B[:, h * 512:(h + 1) * 512], ps, [pk], ["sh1B"]), modT[:, 0:8], "modT")
            if "S" not in phases:
                P.barrier()

        def front_tile(t, xt, hb, tmp, hT_view, col, hkey, ss, rstd, junk, fbank=6, evac="act", addeng="pool"):
            sl = t % len(xt) if xt[0] is not xt[1] else 0
            if isinstance(hb, list):
                d2 = t % 2
                hb, tmp, ss, rstd = hb[d2], tmp[d2], ss[d2], rstd[d2]
                junk = tmp
                kh, kt_, ks, kr = f"hb{d2}", f"tmp{d2}", f"ss{d2}", f"rstd{d2}"
            else:
                kh, kt_, ks, kr = "hb", "tmp", "ss", "rstd"
            dma("sp", xt[sl], xs[t * 128:(t + 1) * 128, :], w=[f"xt{sl}"], dsem=f"xt{sl}")
            act(junk, xt[sl], AF.Square, [f"xt{sl}"], [kt_, ks], accum=ss)
            act(rstd, ss, AF.Ln, [ks, "eps"], [kr], bias=eps_t, scale=1.0 / D)
            act(rstd, rstd, AF.Exp, [kr], [kr], scale=-0.5)
            stt("dve", tmp, xt[sl], rstd, geff1, ALU.mult, ALU.mult, [f"xt{sl}", kr, "geff1"], [kt_])
            tt(addeng, hb, tmp, sh1B, ALU.add, [kt_, "sh1B"], [kh])
            pb = PS[fbank].bitcast(BF16)
            for k in range(8):
                tr(pb[:, k * 128:(k + 1) * 128], hb[:, k * 128:(k + 1) * 128], identb, [kh, "identb"], [PSK[fbank]])
            cp(evac, hT_view[:, :, col:col + 128], pb.rearrange("p (k n) -> p k n", k=8), [PSK[fbank]], [hkey])

        if "S" in phases:
            mS = A.off
            Wl = A.bf16(4 * 2 * 8 * 128).rearrange("p (f r j c) -> p f r j c", f=4, r=2, j=8)
            Cw = A.bf16(16 * 2 * 32).rearrange("p (g r c) -> p g r c", g=16, r=2)
            Cs = A.bf16(16 * 2 * 8 * 32).rearrange("p (g r s c) -> p g r s c", g=16, r=2, s=8)
            r8S = A.f32(16); phiS = A.f32(16); phi32S = A.f32(16)
            Ia = A.f32(512); Ib = A.f32(512)
            quarter = A.f32(1); halfpi = A.f32(1)

            uT = A.bf16(4 * NTOK).rearrange("p (f n) -> p f n", f=4)
            dskip = A.f32(4)
            bglu = A.f32(4)
            mP1 = A.off
            xt0_ = A.f32(D)
            xt = [xt0_, xt0_]
            hb = A.bf16(D)
            tmp = A.f32(D)
            junk = tmp
            ss = A.f32(1)
            rstd = A.f32(1)
            hT0_ = A.bf16(8 * 512).rearrange("p (k n) -> p k n", k=8)
            hT = [hT0_, hT0_]
            winu = A.bf16(8 * 512).rearrange("p (k n) -> p k n", k=8)
            mP1end = A.off
            for k in range(8):
                dma("pool", winu[:, k, :], w_in[k * 128:(k + 1) * 128, 0:512], w=["winu"], dsem="winu")
            load_T(dskip, s5_d.rearrange("(f p) -> f p", p=128), 4, "dskip")
            load_T(bglu, b_glu.rearrange("(f p) -> f p", p=128), 4, "bglu")
            mSetup = A.off
            def s5_setup_gen():

                def coef(lr, li, ls, n, key):
                    ar = A.f32(n); ai = A.f32(n); cfr = A.f32(n); cfi = A.f32(n)
                    m1 = A.off
                    step = A.f32(n); mag = A.f32(n); th = A.f32(n); tf = A.f32(n); ti = A.i32(n); sn = A.f32(n); cs = A.f32(n)
                    t1 = A.f32(n); t2 = A.f32(n); den = A.f32(n)
                    k = lambda s: f"{key}_{s}"
                    act(step, ls, AF.Exp, [k("ls")], [k("step")])
                    tt("dve", mag, lr, step, ALU.mult, [k("lr"), k("step")], [k("mag")])
                    act(mag, mag, AF.Exp, [k("mag")], [k("mag")])
                    tt("dve", th, li, step, ALU.mult, [k("li"), k("step")], [k("th")])
                    sin_of("dve", sn, th, tf, ti, 0.0, [k("th")], [k("sn")])
                    sin_of("dve", cs, th, tf, ti, math.pi / 2, [k("th")], [k("cs")])
                    tt("dve", ar, mag, cs, ALU.mult, [k("mag"), k("cs")], [k("ar")])
                    tt("dve", ai, mag, sn, ALU.mult, [k("mag"), k("sn")], [k("ai")])
                    ts("dve", t1, ar, -1.0, ALU.add, [k("ar")], [k("t1")])
                    tt("dve", den, lr, lr, ALU.mult, [k("lr")], [k("den")])
                    tt("dve", t2, li, li, ALU.mult, [k("li")], [k("t2")])
                    tt("dve", den, den, t2, ALU.add, [k("den"), k("t2")], [k("den")])
                    P.op("dve", lambda e: e.reciprocal(out=den, in_=den), r=[k("den")], w=[k("den")])
                    tt("dve", cfr, t1, lr, ALU.mult, [k("t1"), k("lr")], [k("cfr")])
                    tt("dve", t2, ai, li, ALU.mult, [k("ai"), k("li")], [k("t2")])
                    tt("dve", cfr, cfr, t2, ALU.add, [k("cfr"), k("t2")], [k("cfr")])
                    tt("dve", cfr, cfr, den, ALU.mult, [k("cfr"), k("den")], [k("cfr")])
                    tt("dve", cfi, ai, lr, ALU.mult, [k("ai"), k("lr")], [k("cfi")])
                    tt("dve", t2, t1, li, ALU.mult, [k("t1"), k("li")], [k("t2")])
                    tt("dve", cfi, cfi, t2, ALU.subtract, [k("cfi"), k("t2")], [k("cfi")])
                    tt("dve", cfi, cfi, den, ALU.mult, [k("cfi"), k("den")], [k("cfi")])
                    return ar, ai, cfr, cfi

                def cmul(outr, outi, ar_, ai_, br_, bi_, t1, t2, keys_r, key_w, tk1, tk2):
                    tt("dve", outr, ar_, br_, ALU.mult, keys_r, [key_w + "r"])
                    tt("dve", t1, ai_, bi_, ALU.mult, keys_r, [tk1])
                    tt("dve", outr, outr, t1, ALU.subtract, [key_w + "r", tk1], [key_w + "r"])
                    tt("dve", outi, ar_, bi_, ALU.mult, keys_r, [key_w + "i"])
                    tt("dve", t2, ai_, br_, ALU.mult, keys_r, [tk2])
                    tt("dve", outi, outi, t2, ALU.add, [key_w + "i", tk2], [key_w + "i"])

                lrK = A.f32(512); liK = A.f32(512); lsK = A.f32(512); lsSm = A.f32(8)
                for q in range(4):
                    dma("sp", lrK[32 * q:32 * q + 32, :].rearrange("p (f c) -> p f c", f=4), dram_ap(lam_re, q * 128, [[0, 32], [512, 4], [1, 128]]), w=["K_lr"])
                    dma("sp", liK[32 * q:32 * q + 32, :].rearrange("p (f c) -> p f c", f=4), dram_ap(lam_im, q * 128, [[0, 32], [512, 4], [1, 128]]), w=["K_li"])
                    dma("sp", lsSm[32 * q:32 * q + 32, :].rearrange("p (f t) -> p f t", f=4),
                        dram_ap(log_step, q * 2, [[0, 32], [8, 4], [1, 2]]), w=["lsSm"], slow=True)
                cp("dve", lsK.rearrange("p (f t c) -> p f t c", f=4, t=2), lsSm.rearrange("p (f t) -> p f t", f=4).unsqueeze(3).broadcast_to([128, 4, 2, 64]), ["lsSm"], ["K_ls"])
                yield
                arK, aiK, cfrK, cfiK = coef(lrK, liK, lsK, 512, "K")
                yield
                Bre = A.f32(512); Bim = A.f32(512)
                Bst = [A.f32(512), A.f32(512)]
                for ai_, (src, dst, kk) in enumerate(((s5_b_re, Bre, "Bre"), (s5_b_im, Bim, "Bim"))):
                    bst = Bst[ai_]
                    memset("pool", bst, 0.0, [f"Bst{ai_}"])
                    bst4 = bst.rearrange("p (f b h) -> p f b h", f=4, b=8)
                    for two in range(2):
                        for f_ in range(4):
                            dma("sp" if ai_ == 0 else "act", bst4[64 * two:64 * two + 64, f_, two::2, :],
                                dram_ap(src, (8 * f_ + two) * 1024, [[16, 64], [2048, 4], [1, 16]]), w=[f"Bst{ai_}"], dsem=f"bst{ai_}")
                    bk = nb()
                    for f_ in range(4):
                        tr(PS[bk][:, f_ * 128:(f_ + 1) * 128], bst[:, f_ * 128:(f_ + 1) * 128], identf, [f"Bst{ai_}", "identf"], [PSK[bk]])
                    cp("act", dst, PS[bk], [PSK[bk]], [kk])
                Kr = [A.f32(512), A.f32(512)]; Ki = [A.f32(512), A.f32(512)]
                t1K = A.f32(512); t2K = A.f32(512); t3K = A.f32(512)
                cp("dve", Kr[0], cfrK, ["K_cfr"], ["K0r"])
                cp("dve", Ki[0], cfiK, ["K_cfi"], ["K0i"])
                for j in range(8):
                    a = j % 2
                    kr, ki = Kr[a], Ki[a]
                    krk, kik = f"K{a}r", f"K{a}i"
                    tt("dve", t1K, kr, Bre, ALU.mult, [krk, "Bre"], ["t1K"])
                    tt("dve", t2K, ki, Bim, ALU.mult, [kik, "Bim"], ["t2K"])
                    tt("dve", Wl[:, :, 0, j, :], t1K.rearrange("p (f c) -> p f c", f=4), t2K.rearrange("p (f c) -> p f c", f=4), ALU.subtract, ["t1K", "t2K"], ["Wl"])
                    tt("dve", t1K, kr, Bim, ALU.mult, [krk, "Bim"], ["t1K"])
                    tt("dve", t2K, ki, Bre, ALU.mult, [kik, "Bre"], ["t2K"])
                    tt("dve", Wl[:, :, 1, j, :], t1K.rearrange("p (f c) -> p f c", f=4), t2K.rearrange("p (f c) -> p f c", f=4), ALU.add, ["t1K", "t2K"], ["Wl"])
                    if j < 7:
                        b = 1 - a
                        cmul(Kr[b], Ki[b], kr, ki, arK, aiK, t1K, t3K, [krk, kik, "K_ar", "K_ai"], f"K{b}", "t1K", "t3K")
                    yield

                mSL = A.off
                lrS = A.f32(16); liS = A.f32(16); lsS = A.f32(16)
                load_T(lrS, lam_re.rearrange("(g t) p -> g (t p)", t=2), 16, "S_lr")
                load_T(liS, lam_im.rearrange("(g t) p -> g (t p)", t=2), 16, "S_li")
                lsm2 = A.f32(2)
                lsw = A.f32(128)
                dma("sp", lsm2[0:16, :], log_step.rearrange("(g t) -> g t", t=2), w=["lsm2"])
                cp("dve", lsw[0:16, :].rearrange("p (t c) -> p t c", t=2), lsm2[0:16, :].unsqueeze(2).broadcast_to([16, 2, 64]), ["lsm2"], ["lsw"])
                bk = nb()
                mm(PS[bk][:, 0:16], lsw[0:16, :], identf[0:16, 0:16], True, True, ["lsw", "identf"], [PSK[bk]])
                cp("act", lsS, PS[bk][:, 0:16], [PSK[bk]], ["S_ls"])
                yield
                arS, aiS, _, _ = coef(lrS, liS, lsS, 16, "S")
                yield
                PWre = A.f32(9 * 16).rearrange("p (k g) -> p k g", k=9)
                PWim = A.f32(9 * 16).rearrange("p (k g) -> p k g", k=9)
                t1S = A.f32(16); t2S = A.f32(16)
                memset("dve", PWre[:, 0, :], 1.0, ["PW0r"])
                memset("dve", PWim[:, 0, :], 0.0, ["PW0i"])
                cp("dve", PWre[:, 1, :], arS, ["S_ar"], ["PW1r"])
                cp("dve", PWim[:, 1, :], aiS, ["S_ai"], ["PW1i"])
                for j in range(2, 9):
                    cmul(PWre[:, j, :], PWim[:, j, :], PWre[:, j - 1, :], PWim[:, j - 1, :], arS, aiS, t1S, t2S,
                         [f"PW{j - 1}r", f"PW{j - 1}i", "S_ar", "S_ai"], f"PW{j}", "t1S", "t2S")
                st8 = A.f32(16); tS_a = A.f32(16); t32 = A.f32(16); tfS = A.f32(16); tiS = A.i32(16)
                act(st8, lsS, AF.Exp, ["S_ls"], ["st8"])
                tt("dve", tS_a, lrS, st8, ALU.mult, ["S_lr", "st8"], ["tS_a"])
                act(r8S, tS_a, AF.Exp, ["tS_a"], ["r8S"], scale=8.0)
                tt("dve", phiS, liS, st8, ALU.mult, ["S_li", "st8"], ["phiS"])
                ts("dve", phiS, phiS, 8.0, ALU.mult, ["phiS"], ["phiS"])
                ts("dve", t32, phiS, 32.0, ALU.mult, ["phiS"], ["t32"])
                ts("dve", tfS, t32, 1.0 / TWO_PI, ALU.mult, ["t32"], ["tfS"])
                cp("dve", tiS, tfS, ["tfS"], ["tiS"])
                cp("dve", tfS, tiS, ["tiS"], ["tfS"])
                stt("dve", phi32S, tfS, -TWO_PI, t32, ALU.mult, ALU.add, ["tfS", "t32"], ["phi32S"])
                ones512 = t3K; cidx = A.f32(512); tiI = A.i32(512)
                memset("dve", ones512, 1.0, ["t3K"])
                memset("dve", quarter, 0.25, ["quarter"])
                memset("dve", halfpi, math.pi / 2, ["halfpi"])
                P.op("dve", lambda e: e.tensor_tensor_scan(out=cidx, data0=ones512, data1=ones512, initial=-1.0, op0=ALU.mult, op1=ALU.add),
                     r=["t3K"], w=["cidx"], cost=1.2)
                ts("dve", Ia, cidx, -15.5, ALU.add, ["cidx"], ["Ia"], s2=1.0 / 32, op1=ALU.mult)
                cp("dve", tiI, Ia, ["Ia"], ["tiI"])
                cp("dve", Ia, tiI, ["tiI"], ["Ia"])
                stt("dve", Ib, Ia, -32.0, cidx, ALU.mult, ALU.add, ["Ia", "cidx"], ["Ib"])
                C0re = A.f32(16 * 32); C0im = A.f32(16 * 32)
                for ai_, (src, dst, kk) in enumerate(((s5_c_re, C0re, "C0re"), (s5_c_im, C0im, "C0im"))):
                    cst = Bst[ai_]
                    memset("pool", cst, 0.0, [f"Bst{ai_}"])
                    cst3 = cst.rearrange("p (j c) -> p j c", j=4)
                    for b8 in range(8):
                        dma("sp" if ai_ == 0 else "act", cst3[16 * b8:16 * b8 + 16, :, 64 * (b8 % 2):64 * (b8 % 2) + 64],
                            dram_ap(src, b8 * 1024, [[64, 16], [8192, 4], [1, 64]]), w=[f"Bst{ai_}"], dsem=f"bst{ai_}")
                    bk = nb()
                    for j_ in range(4):
                        tr(PS[bk][:, j_ * 128:(j_ + 1) * 128], cst[:, j_ * 128:(j_ + 1) * 128], identf, [f"Bst{ai_}", "identf"], [PSK[bk]])
                    cp("act", dst, PS[bk], [PSK[bk]], [kk])
                C0re3 = C0re.rearrange("p (g c) -> p g c", g=16)
                C0im3 = C0im.rearrange("p (g c) -> p g c", g=16)
                cp("dve", Cw[:, :, 0, :], C0re3, ["C0re"], ["Cw"])
                ts("dve", Cw[:, :, 1, :], C0im3, -1.0, ALU.mult, ["C0im"], ["Cw"])
                tA = t1K; tB = t2K
                tA3 = tA.rearrange("p (g c) -> p g c", g=16); tB3 = tB.rearrange("p (g c) -> p g c", g=16)
                for s in range(8):
                    a_bc = PWre[:, s + 1, :].unsqueeze(2).broadcast_to([128, 16, 32])
                    b_bc = PWim[:, s + 1, :].unsqueeze(2).broadcast_to([128, 16, 32])
                    kr_ = [f"PW{s + 1}r", f"PW{s + 1}i", "C0re", "C0im"]
                    tt("dve", tA3, C0re3, a_bc, ALU.mult, kr_, ["t1K"])
                    tt("dve", tB3, C0im3, b_bc, ALU.mult, kr_, ["t2K"])
                    tt("dve", Cs[:, :, 0, s, :], tA3, tB3, ALU.subtract, ["t1K", "t2K"], ["Cs"])
                    tt("dve", tA3, C0re3, b_bc, ALU.mult, kr_, ["t1K"])
                    tt("dve", tB3, C0im3, a_bc, ALU.mult, kr_, ["t2K"])
                    tt("dve", tA3, tA3, tB3, ALU.add, ["t1K", "t2K"], ["t1K"])
                    ts("dve", Cs[:, :, 1, s, :], tA3, -1.0, ALU.mult, ["t1K"], ["Cs"])
                    yield
                yield
            def s5_pass1_gen():
                for grp in range(8 if int(os.environ.get("SSTOP", "9")) >= 1 else 0):
                    hs = grp % 2
                    for t4 in range(4):
                        front_tile(grp * 4 + t4, xt, hb, tmp, hT[hs], t4 * 128, f"hT{hs}", ss, rstd, junk, fbank=6 + (grp * 4 + t4) % 2, evac="dve", addeng="dve")
                        yield
                    for f in range(4):
                        bank = f % 4
                        for k in range(8):
                            mm(PS[bank], winu[:, k, f * 128:(f + 1) * 128], hT[hs][:, k, :], k == 0, k == 7, ["winu", f"hT{hs}"], [PSK[bank]])
                        dst = uT[:, f, grp * 512:(grp + 1) * 512]
                        if grp < 4:
                            act(dst, PS[bank], AF.Copy, [PSK[bank], "flag"], ["uT"], scale=flag)
                        else:
                            cp("act", dst, PS[bank], [PSK[bank]], ["uT"])
                        yield


                yield


            for _ in s5_setup_gen():
                pass
            P.barrier("S_setup")
            A.off = mSetup
            xt = [xt0_, A.f32(D), A.f32(D), A.f32(D)]
            hb = [hb, A.bf16(D)]
            tmp = [tmp, A.f32(D)]
            ss = [ss, A.f32(1)]
            rstd = [rstd, A.f32(1)]
            hT = [hT0_, A.bf16(8 * 512).rearrange("p (k n) -> p k n", k=8)]
            for _ in s5_pass1_gen():
                pass
            P.barrier("S_p1")
            A.off = mP1
            Xprev = A.bf16(16 * 2 * 256).rearrange("p (g r n) -> p g r n", g=16, r=2)
            wglu = A.bf16(4 * 512).rearrange("p (k n) -> p k n", k=4)
            for k in range(4):
                dma("pool", wglu[:, k, :], w_glu[k * 128:(k + 1) * 128, :], w=["wglu"], dsem="wglu")
            mP1 = A.off

            sstop = int(os.environ.get("SSTOP", "9"))
            _sb = [0]

            def nb_s():
                _sb[0] += 1
                return 6 + _sb[0] % 2
            A.off = mP1
            mG = A.off
            Gs = [[A.f32(512), A.f32(512)] for _ in range(4)]
            ang = A.f32(512); angb = A.f32(512)
            tfA = A.f32(512); tiA = A.i32(512); tfB = A.f32(512); tiB = A.i32(512)
            cosT = A.f32(512); sinT = A.f32(512)
            p1 = A.f32(512); p2 = A.f32(512); p3 = A.f32(512); p4 = A.f32(512)
            wre = A.f32(512); wim = A.f32(512); zre = A.f32(512); zim = A.f32(512)
            r8t = A.f32(512)
            AY = Arena(arena_t[:, yT_mark + 4096:after_yT], 4096)
            dbl = {"cosT": [cosT, AY.f32(512)], "sinT": [sinT, AY.f32(512)], "p1": [p1, AY.f32(512)], "p2": [p2, AY.f32(512)],
                   "p3": [p3, AY.f32(512)], "p4": [p4, AY.f32(512)], "wre": [wre, AY.f32(512)], "wim": [wim, AY.f32(512)]}

            def mm_bundle(specs, r, w, cost):
                def fn(e):
                    ins = None
                    for (o_, l_, r_, st_, sp_, tp_) in specs:
                        ins = e.matmul(o_, lhsT=l_, rhs=r_, start=st_, stop=sp_, tile_position=tp_)
                    return ins
                return P.op("pe", fn, r=r, w=w, cost=cost)

            for f in range(4 if sstop >= 2 else 0):
                for ri in range(2):
                    banks = [4 * ri + q for q in range(4)]
                    for s_ in range(8):
                        specs = [(PS[banks[q]], Wl[32 * q:32 * q + 32, f, ri, 7 - s_, :], uT[32 * q:32 * q + 32, f, s_::8], s_ == 0, s_ == 7, (32 * q, 0))
                                 for q in range(4)]
                        mm_bundle(specs, ["Wl", "uT"], [PSK[b] for b in banks], 0.95)
                    for q in range(4):
                        cp("act", Gs[q][ri], PS[banks[q]], [PSK[banks[q]]], [f"G{q}{ri}"])
                for q in range(4):
                    gp = f * 4 + q
                    Gre, Gim = Gs[q]
                    gk = [f"G{q}0", f"G{q}1"]
                    pb_ = gp % 2
                    cosT, sinT, p1, p2, p3, p4, wre, wim = (dbl[n_][pb_] for n_ in ("cosT", "sinT", "p1", "p2", "p3", "p4", "wre", "wim"))
                    kC, kS, k1, k2, k3, k4, kwr, kwi = (f"{n_}{pb_}" for n_ in ("cosT", "sinT", "p1", "p2", "p3", "p4", "wre", "wim"))
                    act(angb, Ib, AF.Copy, ["Ib", "phiS"], ["angb"], scale=phiS[:, gp:gp + 1])
                    stt("dve", ang, Ia, phi32S[:, gp:gp + 1], angb, ALU.mult, ALU.add, ["Ia", "phi32S", "angb"], ["ang"])
                    act(r8t, Ia, AF.Identity, ["Ia", "r8S"], ["r8t"], scale=0.0, bias=r8S[:, gp:gp + 1])
                    act(tfA, ang, AF.Copy, ["ang"], ["tfA"], scale=1.0 / TWO_PI)
                    cp("dve", tiA, tfA, ["tfA"], ["tiA"])
                    cp("dve", tfA, tiA, ["tiA"], ["tfA"])
                    stt("dve", tfA, tfA, -TWO_PI, ang, ALU.mult, ALU.add, ["tfA", "ang"], ["tfA"])
                    ts("dve", tfA, tfA, -PI_LO, ALU.max, ["tfA"], ["tfA"], s2=PI_LO, op1=ALU.min)
                    act(sinT, tfA, AF.Sin, ["tfA"], [kS])
                    act(tfB, tfA, AF.Abs, ["tfA"], ["tfB"])
                    act(cosT, tfB, AF.Sin, ["tfB", "halfpi"], [kC], bias=halfpi, scale=-1.0)
                    tt("pool", p1, Gre, cosT, ALU.mult, gk + [kC], [k1])
                    tt("pool", p2, Gim, sinT, ALU.mult, gk + [kS], [k2])
                    tt("pool", p3, Gim, cosT, ALU.mult, gk + [kC], [k3])
                    tt("pool", p4, Gre, sinT, ALU.mult, gk + [kS], [k4])
                    tt("dve", wre, p1, p2, ALU.add, [k1, k2], [kwr])
                    tt("dve", wim, p3, p4, ALU.subtract, [k3, k4], [kwi])
                    P.op("dve", lambda e, wre=wre: e.tensor_tensor_scan(out=zre, data0=r8t, data1=wre, initial=0.0, op0=ALU.mult, op1=ALU.add),
                         r=["r8t", kwr], w=["zre"], cost=1.2)
                    P.op("dve", lambda e, wim=wim: e.tensor_tensor_scan(out=zim, data0=r8t, data1=wim, initial=0.0, op0=ALU.mult, op1=ALU.add),
                         r=["r8t", kwi], w=["zim"], cost=1.2)
                    cs_ = slice(255, 511)
                    tt("pool", p1[:, cs_], zre[:, cs_], cosT[:, cs_], ALU.mult, ["zre", kC], [k1])
                    tt("pool", p2[:, cs_], zim[:, cs_], sinT[:, cs_], ALU.mult, ["zim", kS], [k2])
                    tt("pool", p3[:, cs_], zre[:, cs_], sinT[:, cs_], ALU.mult, ["zre", kS], [k3])
                    tt("pool", p4[:, cs_], zim[:, cs_], cosT[:, cs_], ALU.mult, ["zim", kC], [k4])
                    tt("dve", Xprev[:, gp, 0, :], p1[:, cs_], p2[:, cs_], ALU.subtract, [k1, k2], [f"Xprev{f}"])
                    tt("dve", Xprev[:, gp, 1, :], p3[:, cs_], p4[:, cs_], ALU.add, [k3, k4], [f"Xprev{f}"])

            xtb = [[A.bf16(512) for _ in range(2)] for _ in range(4)]
            yg = A.f32(4 * 512).rearrange("p (f n) -> p f n", f=4)
            ygb = A.bf16(4 * 512).rearrange("p (f n) -> p f n", f=4)
            sig = A.f32(512)
            for ct in range(4 if sstop >= 3 else 0):
                tok0 = NOWN + ct * 512
                for f in range(4):
                    ybank = f % 2
                    yps = PS[ybank]
                    ypsv = yps.rearrange("p (c s) -> p c s", s=8)
                    for ri in range(2):
                        banks = [2 + q for q in range(4)]
                        for j in range(8):
                            specs = []
                            for q in range(4):
                                xv = PS[banks[q]].rearrange("p (c s) -> p c s", s=8)
                                uv = uT[32 * q:32 * q + 32, f, tok0:tok0 + 512].rearrange("p (c s) -> p c s", s=8)
                                specs.append((xv[:, :, j:8], Wl[32 * q:32 * q + 32, f, ri, j, :], uv[:, :, 0:8 - j], j == 0, j == 7, (32 * q, 0)))
                            mm_bundle(specs, ["Wl", "uT"], [PSK[b] for b in banks], 0.6)
                        for q in range(4):
                            cp("act" if q % 2 == 0 else "dve", xtb[q][ri], PS[banks[q]], [PSK[banks[q]]], [f"xtb{q}{ri}"])
                        specs = [(yps[32 * q:32 * q + 32, :], Cw[:, f * 4 + q, ri, :], xtb[q][ri], ri == 0, False, (0, 32 * q)) for q in range(4)]
                        mm_bundle(specs, ["Cw"] + [f"xtb{q}{ri}" for q in range(4)], [PSK[ybank]], 0.5)
                    for s in range(8):
                        for ri in range(2):
                            specs = [(ypsv[32 * q:32 * q + 32, :, s], Cs[:, f * 4 + q, ri, s, :], Xprev[:, f * 4 + q, ri, ct * 64:(ct + 1) * 64],
                                      False, (s == 7 and ri == 1), (0, 32 * q)) for q in range(4)]
                            mm_bundle(specs, ["Cs", f"Xprev{f}"], [PSK[ybank]], 0.3)
                    stt("dve", yg[:, f, :], uT[:, f, tok0:tok0 + 512], dskip[:, f:f + 1], yps, ALU.mult, ALU.add, ["uT", "dskip", PSK[ybank]], ["yg"])
                    act(yg[:, f, :], yg[:, f, :], AF.Gelu_apprx_tanh, ["yg"], ["yg"])
                    cp("pool", ygb[:, f, :], yg[:, f, :], ["yg"], ["ygb"])
                for mt in range(4):
                    bank = 6 + mt % 2
                    for kt in range(4):
                        mm(PS[bank], wglu[:, kt, mt * 128:(mt + 1) * 128], ygb[:, kt, :], kt == 0, kt == 3, ["wglu", "ygb"], [PSK[bank]])
                    act(sig, PS[bank], AF.Sigmoid, [PSK[bank], "bglu"], ["sig"], bias=bglu[:, mt:mt + 1])
                    tt("dve", yT[:, mt, ct * 512:(ct + 1) * 512], yg[:, mt, :], sig, ALU.mult, ["yg", "sig"], ["yT"])
            P.barrier("S_p23")
            A.off = mS


        if "G" in phases:
            mGd = A.off
            xt = [A.f32(D), A.f32(D)]
            hb = A.bf16(D)
            tmp = A.f32(D)
            junk = tmp
            ss = A.f32(1)
            rstd = A.f32(1)
            hT1 = [A.bf16(8 * 128).rearrange("p (k n) -> p k n", k=8) for _ in range(2)]
            Wr = A.bf16(8 * 2056).rearrange("p (k n) -> p k n", k=8)
            pre = A.f32(12 * 131).rearrange("p (f n) -> p f n", f=12)
            cv = A.f32(12 * 128).rearrange("p (f n) -> p f n", f=12)
            qkv = A.f32(8 * 128)
            sq = A.f32(8 * 128)
            rn = A.f32(8 * 128)
            qn = A.f32(512); kn = A.f32(512)
            cwT = A.f32(48)
            ab = A.f32(8)
            zs = A.f32(512)
            gsp = A.f32(4); g_ = A.f32(4); beta = A.f32(4); gmask = A.f32(8); gcl = A.f32(16)
            eg = A.f32(4); ktw = A.f32(4); eglB = A.f32(8); bkeg = A.f32(4)
            dtb = A.f32(4); nae = A.f32(4); gngB = A.f32(128); mA = A.f32(1); mB = A.f32(1)
            Tri = A.f32(128); Bones = A.f32(128); Mst = A.f32(128); Min = A.f32(128)
            M4s = A.f32(512); M4i = A.f32(512)
            diagG = A.f32(512); dmx = A.f32(512); E_ = A.f32(512); EL = A.f32(512); EQ = A.f32(512)
            L_ = A.f32(512); QK = A.f32(512); QKT = A.f32(512)
            Nb = [A.f32(512), A.f32(512)]; Mb = [A.f32(512), A.f32(512)]; Xb = [A.f32(512), A.f32(512)]
            vb = A.f32(512); kbg = A.f32(512); kt = A.f32(512); value = A.f32(512); kcT = A.f32(512)
            vnew = A.f32(512); S_ = A.f32(512); Stmp = A.f32(512); o_t = A.f32(512); otmp = A.f32(512)
            oss = A.f32(4); orstd = A.f32(4); ygd = A.bf16(512)
            tmpP = A.f32(128)

            for k in range(8):
                dma("pool", Wr[:, k, :], w_in[k * 128:(k + 1) * 128, 512:2568], w=["Wr"], dsem="Wr")
            load_T(cwT, conv_w.rearrange("j (f p) -> (j f) p", p=128), 48, "cwT")
            dma("sp", dtb, dt_bias.partition_broadcast(128), w=["dtb"])
            dma("sp", nae, a_log.partition_broadcast(128), w=["nae"])
            dma("sp", gngB, gdn_norm_g.partition_broadcast(128), w=["gngB"])
            act(nae, nae, AF.Exp, ["nae"], ["nae"])
            ts("dve", nae, nae, -1.0, ALU.mult, ["nae"], ["nae"])
            memset("dve", mA, 0.0, ["mA"]); memset("dve", mB, 0.0, ["mB"])
            memset("dve", mA[0:64, :], 1.0, ["mA"]); memset("dve", mB[64:128, :], 1.0, ["mB"])
            memset("pool", Bones, 0.0, ["Bones"])
            memset("pool", Bones[0:64, 0:64], 1.0, ["Bones"])
            memset("pool", Bones[64:128, 64:128], 1.0, ["Bones"])

            def sel(dst, pattern, cm, cmp_op, key):
                cp("pool", dst, Bones, ["Bones"], [key])
                P.op("pool", lambda e: e.affine_select(out=dst, in_=dst, pattern=pattern, compare_op=cmp_op, fill=0.0, base=0,
                                                       channel_multiplier=cm), r=[key], w=[key])
            sel(Tri, [[1, 128]], -1, ALU.is_ge, "Tri")
            sel(Mst, [[-1, 128]], 1, ALU.is_gt, "Mst")
            sel(Min, [[-1, 128]], 1, ALU.is_ge, "Min")
            cp("pool", b3(M4s), Mst.unsqueeze(1).broadcast_to([128, 4, 128]), ["Mst"], ["M4s"])
            cp("pool", b3(M4i), Min.unsqueeze(1).broadcast_to([128, 4, 128]), ["Min"], ["M4i"])
            memset("dve", pre, 0.0, ["pre"])
            memset("dve", S_, 0.0, ["S"])
            ident4 = identf.unsqueeze(1).broadcast_to([128, 4, 128])

            qnS = [qn, A.f32(512), A.f32(512)]
            knS = [kn, A.f32(512), A.f32(512)]
            vvS = [A.f32(512) for _ in range(3)]
            zsS = [zs, A.f32(512), A.f32(512)]
            gsS = [A.f32(48) for _ in range(3)]
            QKTS = [QKT, A.f32(512)]
            ktS = [kt, A.f32(512)]
            valS = [value, A.f32(512)]
            kcTS = [kcT, A.f32(512)]
            qk_ = qkv[:, 0:1024]

            def hs4(h):
                return slice(h * 128, (h + 1) * 128)

            _bc = {"A1": 0, "A2": 0, "B": 0}

            def nbA1():
                _bc["A1"] += 1
                return _bc["A1"] % 2

            def nbA2():
                _bc["A2"] += 1
                return 2 + _bc["A2"] % 3

            def nbB():
                _bc["B"] += 1
                return 5 + _bc["B"] % 3

            def stageA1(t):
                own = t >= 16
                hs = t % 2
                s3 = t % 3
                hk = f"hT1{hs}"
                gs = gsS[s3]
                beta = gs[:, 0:4]; gcl = gs[:, 4:20]; eg = gs[:, 20:24]; ktw = gs[:, 24:28]; eglB = gs[:, 28:36]; bkeg = gs[:, 36:40]
                gsk = f"gs{s3}"
                front_tile(t, xt, hb, tmp, hT1[hs], 0, hk, ss, rstd, junk, fbank=nbA1())
                yield
                for f3 in range(3):
                    bk = nbA1()
                    for f4 in range(4):
                        f = f3 * 4 + f4
                        for k in range(8):
                            mm(PS[bk][:, f4 * 128:(f4 + 1) * 128], Wr[:, k, f * 128:(f + 1) * 128], hT1[hs][:, k, :], k == 0, k == 7, ["Wr", hk], [PSK[bk]])
                    dst = pre[:, f3 * 4:(f3 + 1) * 4, 3:131]
                    if not own:
                        act(dst, b3(PS[bk]), AF.Copy, [PSK[bk], "flag"], ["pre"], scale=flag)
                    else:
                        cp("act", dst, b3(PS[bk]), [PSK[bk]], ["pre"])
                    yield
                bk = nbA1()
                for k in range(8):
                    mm(PS[bk][:, 0:8], hT1[hs][:, k, :], Wr[:, k, 2048:2056], k == 0, k == 7, ["Wr", hk], [PSK[bk]])
                if not own:
                    act(ab, PS[bk][:, 0:8], AF.Copy, [PSK[bk], "flag"], ["ab"], scale=flag)
                else:
                    cp("act", ab, PS[bk][:, 0:8], [PSK[bk]], ["ab"])
                if own:
                    bk = nbA1()
                    for k in range(8):
                        mm(PS[bk], hT1[hs][:, k, :], Wr[:, k, 1536:2048], k == 0, k == 7, ["Wr", hk], [PSK[bk]])
                    act(zsS[s3], PS[bk], AF.Silu, [PSK[bk]], [f"zs{s3}"])
                yield
                for f in range(12):
                    ck = f"cv{f}"
                    act(cv[:, f, :], pre[:, f, 3:131], AF.Copy, ["pre", "cwT"], [ck], scale=cwT[:, 36 + f:36 + f + 1])
                    for j in range(3):
                        stt("dve", cv[:, f, :], pre[:, f, j:j + 128], cwT[:, j * 12 + f:j * 12 + f + 1], cv[:, f, :], ALU.mult, ALU.add, ["pre", "cwT", ck], [ck])
                    if f % 3 == 2:
                        yield
                cp("pool", pre[:, :, 0:3], pre[:, :, 128:131], ["pre"], ["pre"])
                cvf = cv.rearrange("p f n -> p (f n)")
                act(qk_, cvf[:, 0:1024], AF.Silu, [f"cv{f}" for f in range(8)], ["qk"])
                act(vvS[s3], cvf[:, 1024:1536], AF.Silu, [f"cv{f}" for f in range(8, 12)], [f"vv{s3}"])
                yield
                tt("dve", sq, qk_, qk_, ALU.mult, ["qk"], ["sq"])
                for hf in range(2):
                    bk = nbA1()
                    mm(PS[bk], ones, sq[:, hf * 512:(hf + 1) * 512], True, True, ["ones", "sq"], [PSK[bk]])
                    act(rn[:, hf * 512:(hf + 1) * 512], PS[bk], AF.Ln, [PSK[bk], "eps"], ["rn"], bias=eps_t)
                yield
                act(rn, rn, AF.Exp, ["rn"], ["rn"], scale=-0.5)
                stt("dve", qnS[s3], qk_[:, 0:512], 128.0 ** -0.5, rn[:, 0:512], ALU.mult, ALU.mult, ["qk", "rn"], [f"qn{s3}"])
                tt("dve", knS[s3], qk_[:, 512:1024], rn[:, 512:1024], ALU.mult, ["qk", "rn"], [f"kn{s3}"])
                yield
                tt("dve", gsp, ab[:, 0:4], dtb, ALU.add, ["ab", "dtb"], ["gsp"])
                act(gsp, gsp, AF.Exp, ["gsp"], ["gsp"])
                act(gsp, gsp, AF.Ln, ["gsp", "one"], ["gsp"], bias=one_t)
                tt("dve", g_, gsp, nae, ALU.mult, ["gsp", "nae"], ["g"])
                act(beta, ab[:, 4:8], AF.Exp, ["ab"], [gsk], scale=-1.0)
                ts("dve", beta, beta, 1.0, ALU.add, [gsk], [gsk])
                P.op("dve", lambda e, beta=beta: e.reciprocal(out=beta, in_=beta), r=[gsk], w=[gsk])
                yield
                ts("dve", gmask[:, 0:4], g_, mA, ALU.mult, ["g", "mA"], ["gmask"])
                ts("dve", gmask[:, 4:8], g_, mB, ALU.mult, ["g", "mB"], ["gmask"])
                bk = nbA1()
                mm(PS[bk][:, 0:4], Tri, g_, True, True, ["Tri", "g"], [PSK[bk]])
                mm(PS[bk][:, 4:8], Bones, g_, True, True, ["Bones", "g"], [PSK[bk]])
                mm(PS[bk][:, 8:16], ones, gmask, True, True, ["ones", "gmask"], [PSK[bk]])
                cp("dve", gcl, PS[bk][:, 0:16], [PSK[bk]], [gsk])
                yield
                gc = gcl[:, 0:4]; gl = gcl[:, 4:8]; glB = gcl[:, 8:16]
                act(eg, gc, AF.Exp, [gsk], [gsk])
                tt("dve", ktw, gl, gc, ALU.subtract, [gsk], [gsk])
                act(ktw, ktw, AF.Exp, [gsk], [gsk])
                act(eglB, glB, AF.Exp, [gsk], [gsk])
                tt("dve", bkeg, beta, eg, ALU.mult, [gsk], [gsk])
                yield

            def stageA2(t):
                s3 = t % 3
                s2 = t % 2
                gs = gsS[s3]
                beta = gs[:, 0:4]; gcl = gs[:, 4:20]; ktw = gs[:, 24:28]; bkeg = gs[:, 36:40]
                gc = gcl[:, 0:4]
                gsk = f"gs{s3}"
                qn_, kn_, vv_ = qnS[s3], knS[s3], vvS[s3]
                qnk, knk, vvk = f"qn{s3}", f"kn{s3}", f"vv{s3}"
                tt("dve", b3(diagG), ident4, bc_free(gc, 128), ALU.mult, ["identf", gsk], ["diagG"])
                bk = nbA2()
                mm(PS[bk], ones, diagG, True, True, ["ones", "diagG"], [PSK[bk]])
                yield
                for h in range(4):
                    ts("dve", dmx[:, hs4(h)], PS[bk][:, hs4(h)], gc[:, h:h + 1], ALU.subtract, [PSK[bk], gsk], ["dmx"], s2=0.0, op1=ALU.max)
                act(E_, dmx, AF.Exp, ["dmx"], ["E"], scale=-1.0)
                yield
                tt("pool", EL, E_, M4s, ALU.mult, ["E", "M4s"], ["EL"])
                tt("pool", EQ, E_, M4i, ALU.mult, ["E", "M4i"], ["EQ"])
                bkk = nbA2()
                for h in range(4):
                    mm(PS[bkk][:, hs4(h)], kn_[:, hs4(h)], kn_[:, hs4(h)], True, True, [knk], [PSK[bkk]])
                bqk = nbA2()
                for h in range(4):
                    mm(PS[bqk][:, hs4(h)], qn_[:, hs4(h)], kn_[:, hs4(h)], True, True, [qnk, knk], [PSK[bqk]])
                yield
                tt("dve", L_, PS[bkk], EL, ALU.mult, [PSK[bkk], "EL"], ["L"])
                tt("dve", b3(L_), b3(L_), bc_free(beta, 128), ALU.mult, ["L", gsk], ["L"])
                tt("dve", QK, PS[bqk], EQ, ALU.mult, [PSK[bqk], "EQ"], ["QK"])
                yield
                bt = nbA2()
                for h in range(4):
                    tr(PS[bt][:, hs4(h)], L_[:, hs4(h)], identf, ["L", "identf"], [PSK[bt]])
                cp("act", Mb[0], PS[bt], [PSK[bt]], ["M0"])
                bt = nbA2()
                for h in range(4):
                    tr(PS[bt][:, hs4(h)], QK[:, hs4(h)], identf, ["QK", "identf"], [PSK[bt]])
                cp("act", QKTS[s2], PS[bt], [PSK[bt]], [f"QKT{s2}"])
                yield
                tt("dve", b3(Xb[0]), ident4, b3(Mb[0]), ALU.subtract, ["identf", "M0"], ["X0"])
                xc = 0
                Ncur, Nk_ = L_, "L"
                Mcur, Mk_ = Mb[0], "M0"
                for lv in range(1, 6):
                    Nn, Nnk = Nb[lv % 2], f"N{lv % 2}"
                    bn = nbA2()
                    for h in range(4):
                        mm(PS[bn][:, hs4(h)], Mcur[:, hs4(h)], Ncur[:, hs4(h)], True, True, [Mk_, Nk_], [PSK[bn]])
                    cp("act", Nn, PS[bn], [PSK[bn]], [Nnk])
                    if lv < 5:
                        Mn, Mnk = Mb[lv % 2], f"M{lv % 2}"
                        bm = nbA2()
                        for h in range(4):
                            mm(PS[bm][:, hs4(h)], Ncur[:, hs4(h)], Mcur[:, hs4(h)], True, True, [Mk_, Nk_], [PSK[bm]])
                    yield
                    bx = nbA2()
                    for h in range(4):
                        mm(PS[bx][:, hs4(h)], Nn[:, hs4(h)], Xb[xc][:, hs4(h)], True, True, [Nnk, f"X{xc}"], [PSK[bx]])
                    tt("dve", Xb[1 - xc], Xb[xc], PS[bx], ALU.add, [f"X{xc}", PSK[bx]], [f"X{1 - xc}"])
                    xc = 1 - xc
                    if lv < 5:
                        cp("act", Mn, PS[bm], [PSK[bm]], [Mnk])
                        Mcur, Mk_ = Mn, Mnk
                    Ncur, Nk_ = Nn, Nnk
                    yield
                X_, Xk = Xb[xc], f"X{xc}"
                bkt = nbA2()
                for h in range(4):
                    tr(PS[bkt][:, hs4(h)], kn_[:, hs4(h)], identf, [knk, "identf"], [PSK[bkt]])
                bvt = nbA2()
                for h in range(4):
                    tr(PS[bvt][:, hs4(h)], vv_[:, hs4(h)], identf, [vvk, "identf"], [PSK[bvt]])
                yield
                tt("dve", b3(vb), b3(PS[bvt]), bc_free(beta, 128), ALU.mult, [PSK[bvt], gsk], ["vb"])
                tt("dve", b3(kbg), b3(PS[bkt]), bc_free(bkeg, 128), ALU.mult, [PSK[bkt], gsk], ["kbg"])
                tt("dve", b3(ktS[s2]), b3(PS[bkt]), bc_free(ktw, 128), ALU.mult, [PSK[bkt], gsk], [f"kt{s2}"])
                yield
                bval = nbA2()
                for h in range(4):
                    mm(PS[bval][:, hs4(h)], X_[:, hs4(h)], vb[:, hs4(h)], True, True, [Xk, "vb"], [PSK[bval]])
                bkc = nbA2()
                for h in range(4):
                    mm(PS[bkc][:, hs4(h)], kbg[:, hs4(h)], X_[:, hs4(h)], True, True, [Xk, "kbg"], [PSK[bkc]])
                cp("act", valS[s2], PS[bval], [PSK[bval]], [f"val{s2}"])
                cp("act", kcTS[s2], PS[bkc], [PSK[bkc]], [f"kcT{s2}"])
                yield

            def stageB(t):
                own = t >= 16
                s3 = t % 3
                s2 = t % 2
                gs = gsS[s3]
                eg = gs[:, 20:24]; eglB = gs[:, 28:36]
                gsk = f"gs{s3}"
                qn_ = qnS[s3]; qnk = f"qn{s3}"
                QKT_, kt_, value_, kcT_ = QKTS[s2], ktS[s2], valS[s2], kcTS[s2]
                for blk in range(2):
                    r_ = slice(64 * blk, 64 * blk + 64)
                    c0 = 64 * blk
                    bv = nbB()
                    for h in range(4):
                        mm(PS[bv][r_, hs4(h)], kcT_[:, h * 128 + c0:h * 128 + c0 + 64], S_[:, hs4(h)], True, True, [f"kcT{s2}", "S"], [PSK[bv]], tp=(0, c0))
                    tt("dve", vnew[r_, :], value_[r_, :], PS[bv][r_, :], ALU.subtract, [f"val{s2}", PSK[bv]], ["vnew"])
                    yield
                    if own:
                        bo1 = nbB()
                        for h in range(4):
                            mm(PS[bo1][r_, hs4(h)], qn_[:, h * 128 + c0:h * 128 + c0 + 64], S_[:, hs4(h)], True, True, [qnk, "S"], [PSK[bo1]], tp=(0, c0))
                        bo2 = nbB()
                        for h in range(4):
                            mm(PS[bo2][r_, hs4(h)], QKT_[r_, h * 128 + c0:h * 128 + c0 + 64], vnew[r_, hs4(h)], True, True, [f"QKT{s2}", "vnew"], [PSK[bo2]], tp=(c0, c0))
                    bs = nbB()
                    for h in range(4):
                        mm(PS[bs][:, hs4(h)], kt_[r_, hs4(h)], vnew[r_, hs4(h)], True, True, [f"kt{s2}", "vnew"], [PSK[bs]], tp=(c0, 0))
                    tt("dve", b3(Stmp), b3(S_), bc_free(eglB[:, blk * 4:(blk + 1) * 4], 128), ALU.mult, ["S", gsk], ["Stmp"])
                    tt("dve", S_, Stmp, PS[bs], ALU.add, ["Stmp", PSK[bs]], ["S"])
                    yield
                    if own:
                        tt("dve", b3(otmp[r_, :]), b3(PS[bo1][r_, :]), eg[r_, :].unsqueeze(2).broadcast_to([64, 4, 128]), ALU.mult, [PSK[bo1], gsk], ["otmp"])
                        tt("dve", o_t[r_, :], otmp[r_, :], PS[bo2][r_, :], ALU.add, ["otmp", PSK[bo2]], ["o_t"])
                        yield
                if own:
                    act(otmp, o_t, AF.Square, ["o_t"], ["otmp"])
                    P.op("dve", lambda e: e.reduce_sum(out=oss, in_=b3(otmp), axis=AX.X), r=["otmp"], w=["oss"], cost=0.6)
                    act(orstd, oss, AF.Ln, ["oss", "eps"], ["orstd"], bias=eps_t, scale=1.0 / 128)
                    act(orstd, orstd, AF.Exp, ["orstd"], ["orstd"], scale=-0.5)
                    yield
                    tt("dve", b3(otmp), b3(o_t), bc_free(orstd, 128), ALU.mult, ["o_t", "orstd"], ["otmp"])
                    tt("pool", b3(otmp), b3(otmp), gngB.unsqueeze(1).broadcast_to([128, 4, 128]), ALU.mult, ["otmp", "gngB"], ["otmp"])
                    tt("dve", ygd, otmp, zsS[s3], ALU.mult, ["otmp", f"zs{s3}"], ["ygd"])
                    yield
                    bk = nbB()
                    pb = PS[bk].bitcast(BF16)
                    for h in range(4):
                        tr(pb[:, hs4(h)], ygd[:, hs4(h)], identb, ["ygd", "identb"], [PSK[bk]])
                    cp("act", yT[:, 4:8, (t - 16) * 128:(t - 16 + 1) * 128], b3(pb[:, 0:512]), [PSK[bk]], ["yT"])
                    yield

            NT_ = 32
            for it in range(NT_ + 2):
                active = []
                if it < NT_:
                    active.append(stageA1(it))
                if 0 <= it - 1 < NT_:
                    active.append(stageA2(it - 1))
                if 0 <= it - 2 < NT_:
                    active.append(stageB(it - 2))
                while active:
                    for g in list(active):
                        try:
                            next(g)
                        except StopIteration:
                            active.remove(g)
            P.barrier("G")
            A.off = mGd


        if "C" in phases:
            A.off = after_yT
            acc = A.f32(16 * D).rearrange("p (t n) -> p t n", t=16)
            hT2 = A.bf16(8 * NOWN).rearrange("p (k n) -> p k n", k=8)
            comb = A.f32(16 * 32).rearrange("p (t e) -> p t e", t=16)
            gt2B = A.f32(D); gfB = A.f32(D)
            mC1 = A.off
            gt1B = A.f32(D); geff2 = A.f32(D); sh2B = A.f32(D); g2B = A.f32(D)
            diag_slots = [A.f32(128), A.f32(128)]
            wout = A.bf16(8 * D).rearrange("p (k n) -> p k n", k=8)
            wr = A.f32(8 * 36).rearrange("p (k n) -> p k n", k=8)
            xt2 = [A.f32(D), A.f32(D)]
            tmpc = A.f32(D); mf = A.f32(D)
            hTf = A.f32(8 * 128).rearrange("p (k n) -> p k n", k=8)
            ss2 = A.f32(1); rstd2 = A.f32(1)
            lg = A.f32(36); gmax = A.f32(1); oh = A.f32(4); negm = A.f32(1); ex4 = A.f32(4); sum4 = A.f32(1); pg = A.f32(1)
            selv = A.f32(8); sel2 = A.f32(8); m1 = A.f32(1); m2 = A.f32(1); oh1 = A.f32(8); oh2 = A.f32(8)
            dm = A.f32(1); w1 = A.f32(1); w2 = A.f32(1); c8 = A.f32(8)
            dblC = {"lg": [lg, A.f32(36)], "gmax": [gmax, A.f32(1)], "oh": [oh, A.f32(4)], "negm": [negm, A.f32(1)], "ex4": [ex4, A.f32(4)], "sum4": [sum4, A.f32(1)], "pg": [pg, A.f32(1)], "selv": [selv, A.f32(8)], "sel2": [sel2, A.f32(8)], "m1": [m1, A.f32(1)], "m2": [m2, A.f32(1)], "oh1": [oh1, A.f32(8)], "oh2": [oh2, A.f32(8)], "dm": [dm, A.f32(1)], "w1": [w1, A.f32(1)], "w2": [w2, A.f32(1)], "c8": [c8, A.f32(8)], "ss2": [ss2, A.f32(1)], "rstd2": [rstd2, A.f32(1)], "mf": [mf, A.f32(D)]}

            dma("sp", g2B, norm2_g.partition_broadcast(128), w=["g2B"])
            dma("sp", gfB, normf_g.partition_broadcast(128), w=["gfB"])
            for k in range(8):
                dma("pool", wout[:, k, :], w_out[k * 128:(k + 1) * 128, :], w=["wout"], dsem="wout")
                dma("sp", wr[:, k, 0:4], w_rg[k * 128:(k + 1) * 128, :], w=["wr"])
                dma("sp", wr[:, k, 4:36], w_re[k * 128:(k + 1) * 128, :], w=["wr"])
            bcast_row(lambda h, ps, pk: cp("act", gt1B[:, h * 512:(h + 1) * 512], ps, [pk], ["gt1B"]), modT[:, 16:24], "modT")
            bcast_row(lambda h, ps, pk: cp("act", sh2B[:, h * 512:(h + 1) * 512], ps, [pk], ["sh2B"]), modT[:, 24:32], "modT")
            bcast_row(lambda h, ps, pk: stt("dve", geff2[:, h * 512:(h + 1) * 512], ps, 1.0, g2B[:, h * 512:(h + 1) * 512],
                                            ALU.add, ALU.mult, [pk, "g2B"], ["geff2"]), modT[:, 32:40], "modT")
            bcast_row(lambda h, ps, pk: cp("act", gt2B[:, h * 512:(h + 1) * 512], ps, [pk], ["gt2B"]), modT[:, 40:48], "modT")


            cstop = float(os.environ.get("CSTOP", "9"))
            for t in range(16 if cstop >= 2 else 0):
                pp = t % 2
                (lg, gmax, oh, negm, ex4, sum4, pg, selv, sel2, m1, m2, oh1, oh2, dm, w1, w2, c8, ss2, rstd2, mf) = (dblC[n_][pp] for n_ in ('lg', 'gmax', 'oh', 'negm', 'ex4', 'sum4', 'pg', 'selv', 'sel2', 'm1', 'm2', 'oh1', 'oh2', 'dm', 'w1', 'w2', 'c8', 'ss2', 'rstd2', 'mf'))
                sl = t % 2
                dma("sp", xt2[sl], xs[(16 + t) * 128:(17 + t) * 128, :], w=[f"xt2{sl}"], dsem=f"xt2{sl}")
                for nh in range(2):
                    bk = nb()
                    nsl = slice(nh * 512, (nh + 1) * 512)
                    for k in range(8):
                        mm(PS[bk], yT[:, k, t * 128:(t + 1) * 128], wout[:, k, nsl], k == 0, k == 7, ["yT", "wout"], [PSK[bk]])
                    tt("dve", tmpc[:, nsl], PS[bk], gt1B[:, nsl], ALU.mult, [PSK[bk], "gt1B"], ["tmpc"])
                x1 = acc[:, t, :]
                ak = [f"acc{t}0", f"acc{t}1"]
                tt("dve", x1, tmpc, xt2[sl], ALU.add, ["tmpc", f"xt2{sl}"], ak)
                if cstop < 3:
                    continue
                act(tmpc, x1, AF.Square, ak, ["tmpc", f"ss2{pp}"], accum=ss2)
                act(rstd2, ss2, AF.Ln, [f"ss2{pp}", "eps"], [f"rstd2{pp}"], bias=eps_t, scale=1.0 / D)
                act(rstd2, rstd2, AF.Exp, [f"rstd2{pp}"], [f"rstd2{pp}"], scale=-0.5)
                stt("dve", mf, x1, rstd2, geff2, ALU.mult, ALU.mult, ak + [f"rstd2{pp}", "geff2"], [f"mf{pp}"])
                tt("dve", mf, mf, sh2B, ALU.add, [f"mf{pp}", "sh2B"], [f"mf{pp}"])
                if cstop < 3.2:
                    continue
                for half in range(2):
                    bk = nb()
                    for k4 in range(4):
                        k = half * 4 + k4
                        tr(PS[bk][:, k4 * 128:(k4 + 1) * 128], mf[:, k * 128:(k + 1) * 128], identf, [f"mf{pp}", "identf"], [PSK[bk]])
                    if cstop >= 3.4:
                        cp("act", hTf[:, half * 4:(half + 1) * 4, :], b3(PS[bk]), [PSK[bk]], ["hTf"])
                    if cstop >= 3.6:
                        cp("dve", hT2[:, half * 4:(half + 1) * 4, t * 128:(t + 1) * 128], hTf[:, half * 4:(half + 1) * 4, :], ["hTf"], ["hT2"])
                if cstop < 4:
                    continue
                bk = nb()
                for k in range(8):
                    mm(PS[bk][:, 0:36], hTf[:, k, :], wr[:, k, :], k == 0, k == 7, ["hTf", "wr"], [PSK[bk]])
                cp("dve", lg, PS[bk][:, 0:36], [PSK[bk]], [f"lg{pp}"])
                P.op("dve", lambda e, lg=lg, gmax=gmax: e.reduce_max(out=gmax, in_=lg[:, 0:4], axis=AX.X), r=[f"lg{pp}"], w=[f"gmax{pp}"])
                ts("dve", oh, lg[:, 0:4], gmax, ALU.is_equal, [f"lg{pp}", f"gmax{pp}"], [f"oh{pp}"])
                ts("dve", negm, gmax, -1.0, ALU.mult, [f"gmax{pp}"], [f"negm{pp}"])
                act(ex4, lg[:, 0:4], AF.Exp, [f"lg{pp}", f"negm{pp}"], [f"ex4{pp}", f"sum4{pp}"], bias=negm, accum=sum4)
                P.op("dve", lambda e, sum4=sum4, pg=pg: e.reciprocal(out=pg, in_=sum4), r=[f"sum4{pp}"], w=[f"pg{pp}"])
                ts("dve", selv, lg[:, 4:12], oh[:, 0:1], ALU.mult, [f"lg{pp}", f"oh{pp}"], [f"selv{pp}"])
                for g in range(1, 4):
                    stt("dve", selv, lg[:, 4 + 8 * g:12 + 8 * g], oh[:, g:g + 1], selv, ALU.mult, ALU.add, [f"lg{pp}", f"oh{pp}", f"selv{pp}"], [f"selv{pp}"])
                P.op("dve", lambda e, selv=selv, m1=m1: e.reduce_max(out=m1, in_=selv, axis=AX.X), r=[f"selv{pp}"], w=[f"m1{pp}"])
                ts("dve", oh1, selv, m1, ALU.is_equal, [f"selv{pp}", f"m1{pp}"], [f"oh1{pp}"])
                stt("dve", sel2, oh1, -1.0e30, selv, ALU.mult, ALU.add, [f"oh1{pp}", f"selv{pp}"], [f"sel2{pp}"])
                P.op("dve", lambda e, sel2=sel2, m2=m2: e.reduce_max(out=m2, in_=sel2, axis=AX.X), r=[f"sel2{pp}"], w=[f"m2{pp}"])
                ts("dve", oh2, sel2, m2, ALU.is_equal, [f"sel2{pp}", f"m2{pp}"], [f"oh2{pp}"])
                tt("dve", dm, m1, m2, ALU.subtract, [f"m1{pp}", f"m2{pp}"], [f"dm{pp}"])
                act(w1, dm, AF.Exp, [f"dm{pp}"], [f"w1{pp}"], scale=-1.0)
                ts("dve", w1, w1, 1.0, ALU.add, [f"w1{pp}"], [f"w1{pp}"])
                P.op("dve", lambda e, w1=w1: e.reciprocal(out=w1, in_=w1), r=[f"w1{pp}"], w=[f"w1{pp}"])
                ts("dve", w2, w1, -1.0, ALU.mult, [f"w1{pp}"], [f"w2{pp}"], s2=1.0, op1=ALU.add)
                tt("dve", w1, w1, pg, ALU.mult, [f"w1{pp}", f"pg{pp}"], [f"w1{pp}"])
                tt("dve", w2, w2, pg, ALU.mult, [f"w2{pp}", f"pg{pp}"], [f"w2{pp}"])
                ts("dve", c8, oh1, w1, ALU.mult, [f"oh1{pp}", f"w1{pp}"], [f"c8{pp}"])
                stt("dve", c8, oh2, w2, c8, ALU.mult, ALU.add, [f"oh2{pp}", f"w2{pp}", f"c8{pp}"], [f"c8{pp}"])
                for g in range(4):
                    ts("dve", comb[:, t, g * 8:(g + 1) * 8], c8, oh[:, g:g + 1], ALU.mult, [f"c8{pp}", f"oh{pp}"], ["comb"])
            if "x1" in dbg:
                o = nc.dram_tensor("dbg_x1", [128, 16 * D], F32, kind="ExternalOutput").ap()
                dbg_out["x1"] = o
                dma("sp", o, acc.rearrange("p t n -> p (t n)"), r=[f"acc{t}{h}" for t in range(16) for h in range(2)], dsem="dbgst")
                o = nc.dram_tensor("dbg_comb", [128, 16 * 32], F32, kind="ExternalOutput").ap()
                dbg_out["comb"] = o
                dma("sp", o, comb.rearrange("p t n -> p (t n)"), r=["comb"], dsem="dbgst")
            A1 = Arena(arena_t[:, yT_mark:after_yT], after_yT - yT_mark)
            wg = [A1.bf16(8 * 256).rearrange("p (k n) -> p k n", k=8) for _ in range(2)]
            wu = [A1.bf16(8 * 256).rearrange("p (k n) -> p k n", k=8) for _ in range(2)]
            wd = [A1.bf16(2 * D).rearrange("p (f n) -> p f n", f=2) for _ in range(2)]
            n_exp = int(os.environ.get('NEXP', '32'))

            def load_expert(e_, extra_r=()):
                sl = e_ % 2
                dma("pool", wg[sl], w_gate[e_].rearrange("(k p) n -> p k n", p=128), r=list(extra_r), w=[f"wg{sl}"], dsem=f"wg{sl}")
                dma("pool", wu[sl], w_up[e_].rearrange("(k p) n -> p k n", p=128), r=list(extra_r), w=[f"wu{sl}"], dsem=f"wu{sl}")
                dma("pool", wd[sl], w_down[e_].rearrange("(f p) n -> p f n", p=128), r=list(extra_r), w=[f"wd{sl}"], dsem=f"wd{sl}")

            guard = A.f32(1)
            memset("pool", guard, 0.0, ["yT", "yTguard"])
            for e_ in range(min(2, n_exp)):
                load_expert(e_, ["yTguard"])
            P.barrier("C1")
            A.off = mC1
            actT = [A.bf16(2 * NOWN).rearrange("p (f n) -> p f n", f=2) for _ in range(2)]
            sg = [A.f32(512), A.f32(512)]
            ot = [A.f32(D), A.f32(D)]
            sqj = A.f32(D); ss3 = A.f32(1); rstd3 = A.f32(1)

            cnt = 0
            for e_ in range(n_exp):
                sl = e_ % 2
                if e_ >= 2:
                    load_expert(e_)
                tt("pool", wd[sl], wd[sl], gt2B.unsqueeze(1).broadcast_to([128, 2, D]), ALU.mult, [f"wd{sl}", "gt2B"], [f"wd{sl}"])
                for ct in range(4):
                    csl = slice(ct * 512, (ct + 1) * 512)
                    for ft in range(2):
                        fsl = slice(ft * 128, (ft + 1) * 128)
                        bg = nb()
                        for k in range(8):
                            mm(PS[bg], wg[sl][:, k, fsl], hT2[:, k, csl], k == 0, k == 7, [f"wg{sl}", "hT2"], [PSK[bg]])
                        bu = nb()
                        for k in range(8):
                            mm(PS[bu], wu[sl][:, k, fsl], hT2[:, k, csl], k == 0, k == 7, [f"wu{sl}", "hT2"], [PSK[bu]])
                        s2_ = cnt % 2
                        cnt += 1
                        act(sg[s2_], PS[bg], AF.Silu, [PSK[bg]], [f"sg{s2_}"])
                        tt("dve", actT[sl][:, ft, csl], sg[s2_], PS[bu], ALU.mult, [f"sg{s2_}", PSK[bu]], [f"actT{sl}{ct}"])
                for t in range(16):
                    for nh in range(2):
                        nsl = slice(nh * 512, (nh + 1) * 512)
                        bd = nb()
                        for ft in range(2):
                            mm(PS[bd], actT[sl][:, ft, t * 128:(t + 1) * 128], wd[sl][:, ft, nsl], ft == 0, ft == 1,
                               [f"actT{sl}{t // 4}", f"wd{sl}"], [PSK[bd]])
                        stt("dve", acc[:, t, nsl], PS[bd], comb[:, t, e_:e_ + 1], acc[:, t, nsl], ALU.mult, ALU.add,
                            [PSK[bd], "comb", f"acc{t}{nh}"], [f"acc{t}{nh}"])
            for t in range(16):
                sl = t % 2
                ak = [f"acc{t}0", f"acc{t}1"]
                act(sqj, acc[:, t, :], AF.Square, ak, ["sqj", "ss3"], accum=ss3)
                act(rstd3, ss3, AF.Ln, ["ss3", "eps"], ["rstd3"], bias=eps_t, scale=1.0 / D)
                act(rstd3, rstd3, AF.Exp, ["rstd3"], ["rstd3"], scale=-0.5)
                stt("dve", ot[sl], acc[:, t, :], rstd3, gfB, ALU.mult, ALU.mult, ak + ["rstd3", "gfB"], [f"ot{sl}"])
                dma("sp", out_d[t * 128:(t + 1) * 128, :], ot[sl], r=[f"ot{sl}"], dsem="out")

        if "yT" in dbg:
            o = nc.dram_tensor("dbg_yT", [128, 8 * NOWN], F32, kind="ExternalOutput").ap()
            dbg_out["yT"] = o
            ytmp = A.f32(8 * 512)
            yTflat = yT.rearrange("p k n -> p (k n)")
            for c in range(4):
                cp("dve", ytmp, yTflat[:, c * 4096:(c + 1) * 4096], ["yT"], ["ytmp"])
                dma("sp", o[:, c * 4096:(c + 1) * 4096], ytmp, r=["ytmp"], dsem="dbgst")

        for name in list(P.dcount):
            pass
        P.barrier("C2")
        P.emit(st)
    return nc, dbg_out


def _prep_inputs(inputs):
    x = np.ascontiguousarray(inputs["x"], dtype=np.float32)
    shared = {
        "norm1_g": inputs["norm1_g"].reshape(1, D), "norm2_g": inputs["norm2_g"].reshape(1, D),
        "normf_g": inputs["normf_g"].reshape(1, D),
        "w_ada": inputs["w_ada"][0], "b_ada": inputs["b_ada"][0], "w_in": inputs["w_in"][0],
        "lam_re": inputs["lam_re"][0], "lam_im": inputs["lam_im"][0], "log_step": inputs["log_step"][0],
        "s5_b_re": inputs["s5_b_re"][0], "s5_b_im": inputs["s5_b_im"][0],
        "s5_c_re": inputs["s5_c_re"][0], "s5_c_im": inputs["s5_c_im"][0],
        "s5_d": inputs["s5_d"][0], "w_glu": inputs["w_glu"][0], "b_glu": inputs["b_glu"][0],
        "conv_w": inputs["conv_w"][0], "a_log": inputs["a_log"].reshape(1, 4), "dt_bias": inputs["dt_bias"].reshape(1, 4),
        "gdn_norm_g": inputs["gdn_norm_g"].reshape(1, 128), "w_out": inputs["w_out"][0],
        "w_router_grp": inputs["w_router_grp"][0], "w_router_exp": inputs["w_router_exp"][0],
        "w_gate": inputs["w_gate"][0], "w_up": inputs["w_up"][0], "w_down": inputs["w_down"][0],
    }
    shared = {k: np.ascontiguousarray(v, dtype=np.float32) for k, v in shared.items()}
    in_maps = []
    for core in range(8):
        b, half = core // 2, core % 2
        if half == 0:
            xs_ = np.concatenate([np.zeros((NOWN, D), np.float32), x[b, :NOWN]], axis=0)
        else:
            xs_ = x[b]
        m = dict(shared)
        m["xs"] = np.ascontiguousarray(xs_)
        m["flag"] = np.full((128, 1), float(half), np.float32)
        m["cb"] = np.ascontiguousarray(inputs["c"][b:b + 1], dtype=np.float32)
        in_maps.append(m)
    return in_maps


def kernel(**inputs):
    nc, _ = build()
    in_maps = _prep_inputs(inputs)
    res = run_bass_kernel_spmd(nc, in_maps, core_ids=list(range(8)))
    out = np.zeros((4, 4096, D), np.float32)
    for core in range(8):
        b, half = core // 2, core % 2
        out[b, half * NOWN:(half + 1) * NOWN] = res.results[core]["out"]
    return out
```
